# Optimizing a Trainium2 kernel written in Bass

```python
import jax, jax.numpy as jnp
from jax import lax
import numpy as np

D_MODEL = 2048
BATCH = 2
SEQ = 4096
DEPTH = 4

N_META = 16
RET_HEADS = 8
RET_DK = 256
RET_DV = 512
RET_CHUNK = 128
RET_QK = RET_HEADS * RET_DK
RET_V = RET_HEADS * RET_DV
HG_HEADS = 16
HG_DK = 128
HG_DV = 128
HG_CHUNK = 16
HG_K = HG_HEADS * HG_DK
HG_V = HG_HEADS * HG_DV
D_FF = 5632
N_EXPERTS = 8
TOP_K = 2
D_FF_EXPERT = 2816
N_DENSE = (DEPTH + 1) // 2
N_MOE = DEPTH // 2
ROPE_BASE = 10000.0
LN_EPS = 1e-5
DN_ALPHA = (2 * DEPTH) ** 0.25
DN_BETA = (8 * DEPTH) ** -0.25
SPLITS = (RET_QK, RET_QK, RET_V, RET_V, HG_K, HG_K, HG_V, HG_V, D_MODEL, D_MODEL)
P_IN = 24576

kernel_name = 'retnet_hgrn2_gated_merge_deepnorm_moe'


def layer_norm(x, g, b):
    xf = x.astype(jnp.float32)
    mu = xf.mean(-1, keepdims=True)
    var = jnp.square(xf - mu).mean(-1, keepdims=True)
    return ((xf - mu) * lax.rsqrt(var + LN_EPS) * g + b).astype(x.dtype)


def rotary(x, pos):
    half = x.shape[-1] // 2
    inv = 1.0 / (ROPE_BASE ** jnp.linspace(0.0, 1.0, half, dtype=jnp.float32))
    ang = pos[:, None] * inv[None, :]
    cos = jnp.cos(ang)[None, :, None, :]
    sin = jnp.sin(ang)[None, :, None, :]
    x1, x2 = x[..., :half], x[..., half:]
    return jnp.concatenate([x1 * cos - x2 * sin, x1 * sin + x2 * cos], axis=-1)


def retention_branch(q, k, v):
    B, T = q.shape[:2]
    pad = RET_CHUNK - N_META
    padf = lambda a: jnp.pad(a, ((0, 0), (pad, 0), (0, 0), (0, 0)))
    q, k, v = padf(q) * (RET_DK ** -0.5), padf(k), padf(v)
    n = (T + pad) // RET_CHUNK
    to_chunks = lambda a: a.reshape(B, n, RET_CHUNK, RET_HEADS, a.shape[-1]).transpose(1, 0, 3, 2, 4)
    log_gamma = jnp.log1p(-jnp.exp2(-5.0 - jnp.arange(RET_HEADS, dtype=jnp.float32)))
    idx = jnp.arange(RET_CHUNK, dtype=jnp.float32)
    rel = idx[:, None] - idx[None, :]
    intra = jnp.where(rel >= 0, jnp.exp(jnp.maximum(rel, 0.0) * log_gamma[:, None, None]), 0.0)
    q_decay = jnp.exp((idx + 1.0) * log_gamma[:, None])[None, :, :, None]
    k_decay = jnp.exp((RET_CHUNK - 1.0 - idx) * log_gamma[:, None])[None, :, :, None]
    c_decay = jnp.exp(RET_CHUNK * log_gamma)[None, :, None, None]

    def step(R, blk):
        qc, kc, vc = blk
        s = jnp.einsum('bhtd,bhsd->bhts', qc, kc) * intra
        o = jnp.einsum('bhts,bhsv->bhtv', s, vc) + jnp.einsum('bhtd,bhdv->bhtv', qc * q_decay, R)
        R = R * c_decay + jnp.einsum('bhsd,bhsv->bhdv', kc * k_decay, vc)
        return R, o

    R0 = jnp.zeros((B, RET_HEADS, RET_DK, RET_DV), jnp.float32)
    _, o = lax.scan(step, R0, (to_chunks(q), to_chunks(k), to_chunks(v)))
    o = o.transpose(1, 0, 3, 2, 4).reshape(B, n * RET_CHUNK, RET_HEADS, RET_DV)
    return o[:, pad:]


def hgrn2_branch(q, k, v, log_f):
    B, T = q.shape[:2]
    n = T // HG_CHUNK
    to_chunks = lambda a: a.reshape(B, n, HG_CHUNK, HG_HEADS, a.shape[-1]).transpose(1, 0, 3, 2, 4)
    tri = jnp.tril(jnp.ones((HG_CHUNK, HG_CHUNK), dtype=bool))[:, :, None]

    def step(S, blk):
        qc, kc, vc, gc = blk
        b = jnp.cumsum(gc, axis=2)
        b_last = b[:, :, -1:, :]
        o = jnp.einsum('bhtd,bhdv->bhtv', qc * jnp.exp(b), S)
        decay = jnp.exp(jnp.where(tri, b[:, :, :, None, :] - b[:, :, None, :, :], -jnp.inf))
        a = jnp.einsum('bhtsd,bhsd->bhts', qc[:, :, :, None, :] * decay, kc)
        o = o + jnp.einsum('bhts,bhsv->bhtv', a, vc)
        S = S * jnp.exp(b_last[:, :, 0, :, None]) + jnp.einsum('bhsd,bhsv->bhdv', kc * jnp.exp(b_last - b), vc)
        return S, o

    S0 = jnp.zeros((B, HG_HEADS, HG_DK, HG_DV), jnp.float32)
    _, o = lax.scan(step, S0, (to_chunks(q), to_chunks(k), to_chunks(v), to_chunks(log_f)))
    return o.transpose(1, 0, 3, 2, 4).reshape(B, T, HG_HEADS, HG_DV)


def mixer(x, pos, w_in, ret_gn_g, hg_norm_g, hg_lb, w_ret_out, w_hg_out, w_o):
    B, T, _ = x.shape
    f32 = lambda a: a.astype(jnp.float32)
    heads = lambda a, h: f32(a).reshape(B, T, h, -1)
    proj = x @ w_in
    rq, rk, rv, rg, hq, hf, hi, hg, ga, gb = jnp.split(proj, list(np.cumsum(SPLITS)[:-1]), axis=-1)
    o_r = retention_branch(rotary(heads(rq, RET_HEADS), pos), rotary(heads(rk, RET_HEADS), pos),
                           heads(rv, RET_HEADS))
    mu = o_r.mean(-1, keepdims=True)
    var = jnp.square(o_r - mu).mean(-1, keepdims=True)
    o_r = ((o_r - mu) * lax.rsqrt(var + LN_EPS)).reshape(B, T, RET_V) * ret_gn_g
    y_r = (o_r * jax.nn.silu(f32(rg))).astype(x.dtype) @ w_ret_out
    f = hg_lb + (1.0 - hg_lb) * jax.nn.sigmoid(f32(hf))
    o_h = hgrn2_branch(jax.nn.silu(heads(hq, HG_HEADS)),
                       (1.0 - f).reshape(B, T, HG_HEADS, HG_DK),
                       heads(hi, HG_HEADS),
                       jnp.log(f).reshape(B, T, HG_HEADS, HG_DK))
    o_h = (o_h * lax.rsqrt(jnp.square(o_h).mean(-1, keepdims=True) + LN_EPS)).reshape(B, T, HG_V) * hg_norm_g
    y_h = (o_h * jax.nn.silu(f32(hg))).astype(x.dtype) @ w_hg_out
    merged = jax.nn.sigmoid(f32(ga)) * f32(y_r) + jax.nn.sigmoid(f32(gb)) * f32(y_h)
    return merged.astype(x.dtype) @ w_o


def swiglu(x, w_gate, w_up, w_down):
    return (jax.nn.silu(x @ w_gate) * (x @ w_up)) @ w_down


def moe_swiglu(x, w_router, w_gate, w_up, w_down):
    logits = (x @ w_router).astype(jnp.float32)
    top_v, top_i = lax.top_k(logits, TOP_K)
    top_w = jax.nn.softmax(top_v, axis=-1)
    gates = jnp.sum(jax.nn.one_hot(top_i, N_EXPERTS, dtype=jnp.float32) * top_w[..., None], axis=-2)
    y = jnp.zeros_like(x)
    for e in range(N_EXPERTS):
        y = y + gates[..., e:e + 1].astype(x.dtype) * swiglu(x, w_gate[e], w_up[e], w_down[e])
    return y


def setup_inputs(seed: int = 0) -> dict:
    key = jax.random.key(seed)
    ks = jax.random.split(key, 24)
    nrm = lambda k, shape, scale: jax.random.normal(k, shape, jnp.float32) * scale
    return {
        'x': nrm(ks[0], (BATCH, SEQ, D_MODEL), 1.0),
        'meta_tokens': nrm(ks[1], (N_META, D_MODEL), 1.0),
        'w_in': nrm(ks[2], (DEPTH, D_MODEL, P_IN), D_MODEL ** -0.5),
        'ret_gn_g': 1.0 + nrm(ks[3], (DEPTH, RET_V), 0.02),
        'hg_norm_g': 1.0 + nrm(ks[4], (DEPTH, HG_V), 0.02),
        'hg_lb_logits': nrm(ks[5], (DEPTH, HG_K), 0.5),
        'w_ret_out': nrm(ks[6], (DEPTH, RET_V, D_MODEL), RET_V ** -0.5),
        'w_hg_out': nrm(ks[7], (DEPTH, HG_V, D_MODEL), HG_V ** -0.5),
        'w_o': nrm(ks[8], (DEPTH, D_MODEL, D_MODEL), D_MODEL ** -0.5 * DN_BETA),
        'ln1_g': 1.0 + nrm(ks[9], (DEPTH, D_MODEL), 0.02),
        'ln1_b': nrm(ks[10], (DEPTH, D_MODEL), 0.02),
        'ln2_g': 1.0 + nrm(ks[11], (DEPTH, D_MODEL), 0.02),
        'ln2_b': nrm(ks[12], (DEPTH, D_MODEL), 0.02),
        'ffn_w_gate': nrm(ks[13], (N_DENSE, D_MODEL, D_FF), D_MODEL ** -0.5),
        'ffn_w_up': nrm(ks[14], (N_DENSE, D_MODEL, D_FF), D_MODEL ** -0.5),
        'ffn_w_down': nrm(ks[15], (N_DENSE, D_FF, D_MODEL), D_FF ** -0.5 * DN_BETA),
        'moe_router': nrm(ks[16], (N_MOE, D_MODEL, N_EXPERTS), D_MODEL ** -0.5),
        'moe_w_gate': nrm(ks[17], (N_MOE, N_EXPERTS, D_MODEL, D_FF_EXPERT), D_MODEL ** -0.5),
        'moe_w_up': nrm(ks[18], (N_MOE, N_EXPERTS, D_MODEL, D_FF_EXPERT), D_MODEL ** -0.5),
        'moe_w_down': nrm(ks[19], (N_MOE, N_EXPERTS, D_FF_EXPERT, D_MODEL), D_FF_EXPERT ** -0.5 * DN_BETA),
    }


def reference(x, meta_tokens, w_in, ret_gn_g, hg_norm_g, hg_lb_logits, w_ret_out, w_hg_out, w_o,
              ln1_g, ln1_b, ln2_g, ln2_b, ffn_w_gate, ffn_w_up, ffn_w_down,
              moe_router, moe_w_gate, moe_w_up, moe_w_down):
    B = x.shape[0]
    meta = jnp.broadcast_to(meta_tokens[None].astype(x.dtype), (B, N_META, D_MODEL))
    h = jnp.concatenate([meta, x], axis=1)
    pos = jnp.arange(h.shape[1], dtype=jnp.float32)
    lb = jnp.cumsum(jax.nn.softmax(hg_lb_logits.astype(jnp.float32), axis=0), axis=0)
    lb = lb - lb[0:1]
    for l in range(DEPTH):
        m = mixer(h, pos, w_in[l], ret_gn_g[l], hg_norm_g[l], lb[l], w_ret_out[l], w_hg_out[l], w_o[l])
        h = layer_norm(DN_ALPHA * h + m, ln1_g[l], ln1_b[l])
        if l % 2 == 0:
            f = swiglu(h, ffn_w_gate[l // 2], ffn_w_up[l // 2], ffn_w_down[l // 2])
        else:
            f = moe_swiglu(h, moe_router[l // 2], moe_w_gate[l // 2], moe_w_up[l // 2], moe_w_down[l // 2])
        h = layer_norm(DN_ALPHA * h + f, ln2_g[l], ln2_b[l])
    return h[:, N_META:]
```

```python
import numpy as np
import concourse.bass as bass
import concourse.mybir as mybir
from concourse.bass_utils import run_bass_kernel_spmd

F32 = mybir.dt.float32
BF16 = mybir.dt.bfloat16
AF = mybir.ActivationFunctionType
ALU = mybir.AluOpType
AX = mybir.AxisListType

D = 2048
NT = 1152
NTILE = 9
P_IN = 24576
DFF = 5632
DFE = 2816
NE = 8
ALPHA = (2 * 4) ** 0.25
EPS = 1e-5
OFF_RQ, OFF_RK, OFF_RV, OFF_RG = 0, 2048, 4096, 8192
OFF_HQ, OFF_HF, OFF_HI, OFF_HG = 12288, 14336, 16384, 18432
OFF_GA, OFF_GB = 20480, 22528
TB = 576
NTB = 2
HB = 288


_DBG = {}


class Buf:
    def __init__(self, name):
        self.name = name
        self.w = None
        self.r = []
        self.sem = None
        self.semval = 0


class Op:
    __slots__ = ("eng", "fn", "deps", "dma_buf", "needed", "tok")

    def __init__(self, eng, fn, dma_buf=None):
        self.eng = eng
        self.fn = fn
        self.deps = []
        self.dma_buf = dma_buf
        self.needed = False
        self.tok = None


class Prog:
    ENGS = ("pe", "act", "dve", "pool", "sp")
    SEG = 30000

    def __init__(self, nc):
        self.nc = nc
        self.ops = {e: [] for e in self.ENGS}
        self.all_ops = []
        self.bufs = {}
        self.dma_since = []

    def B(self, name):
        b = self.bufs.get(name)
        if b is None:
            b = Buf(name)
            self.bufs[name] = b
        return b

    def emit(self, eng, fn, reads=(), writes=(), dma_buf=None):
        op = Op(eng, fn, dma_buf)
        deps = op.deps
        for b in reads:
            if b.w is not None:
                deps.append(b.w)
        for b in writes:
            if b.w is not None:
                deps.append(b.w)
            deps.extend(b.r)
        for b in reads:
            b.r.append(op)
        for b in writes:
            b.w = op
            b.r = []
        self.ops[eng].append(op)
        self.all_ops.append(op)
        if dma_buf is not None:
            self.dma_since.append(op)
        return op

    def barrier(self):
        deps = []
        for e in self.ENGS:
            for op in reversed(self.ops[e]):
                if op.fn is not None and op.dma_buf is None:
                    deps.append(op)
                    break
        deps.extend(self.dma_since)
        self.dma_since = []
        for e in self.ENGS:
            m = Op(e, None)
            m.deps = list(deps)
            self.ops[e].append(m)
            self.all_ops.append(m)
        for b in self.bufs.values():
            b.w = None
            b.r = []

    def finalize(self, final_wait_ops=()):
        nc = self.nc
        for op in self.all_ops:
            for d in op.deps:
                if d.dma_buf is not None or d.eng != op.eng or op.eng != "pe":
                    d.needed = True
        for op in final_wait_ops:
            op.needed = True
        eng_sems = {e: [] for e in self.ENGS}
        for e in self.ENGS:
            cnt = 0
            for op in self.ops[e]:
                if op.dma_buf is not None:
                    b = op.dma_buf
                    if b.sem is None:
                        b.sem = nc.alloc_semaphore("ds_" + b.name)
                    b.semval += 16
                    op.tok = (b.sem, b.semval, 16)
                elif op.needed:
                    seg = cnt // self.SEG
                    if seg >= len(eng_sems[e]):
                        eng_sems[e].append(nc.alloc_semaphore(f"es_{e}_{seg}"))
                    op.tok = (eng_sems[e][seg], cnt % self.SEG + 1, 1)
                    cnt += 1
        prog = self

        def run_engine(e, engine, extra_final=()):
            known = {}
            for op in prog.ops[e]:
                waits = {}
                for d in op.deps:
                    if d.tok is None:
                        continue
                    if d.dma_buf is None and d.eng == e and e == "pe":
                        continue
                    sem, val, _ = d.tok
                    key = id(sem)
                    if known.get(key, 0) >= val:
                        continue
                    if key not in waits or waits[key][1] < val:
                        waits[key] = (sem, val)
                for key, (sem, val) in waits.items():
                    engine.wait_ge(sem, val)
                    known[key] = val
                if op.fn is None:
                    continue
                ins = op.fn(engine)
                if op.tok is not None:
                    ins.then_inc(op.tok[0], op.tok[2])
            for d in extra_final:
                sem, val, _ = d.tok
                engine.wait_ge(sem, val)

        with nc.Block() as block:
            @block.tensor
            def _(eng):
                run_engine("pe", eng)

            @block.scalar
            def _(eng):
                run_engine("act", eng)

            @block.vector
            def _(eng):
                run_engine("dve", eng)

            @block.gpsimd
            def _(eng):
                run_engine("pool", eng)

            @block.sync
            def _(eng):
                run_engine("sp", eng, extra_final=final_wait_ops)


def build(kind, moe=False, phases=("mix", "tok")):
    nc = bass.Bass("TRN2", target_bir_lowering=False)
    P = Prog(nc)
    Bf = P.B
    cache = {}
    final_ops = []

    def din(name, shape, dt=F32):
        return nc.dram_tensor(name, list(shape), dt, kind="ExternalInput").ap()

    def dout(name, shape, dt=F32):
        return nc.dram_tensor(name, list(shape), dt, kind="ExternalOutput").ap()

    def dscr(name, shape, dt=F32):
        return nc.dram_tensor(name, list(shape), dt, kind="Internal").ap()

    def body(full, moe, dd, stmode, seg):
        hT_d = dd["hT"]; w_in_d = dd["w_in"]; lbl_d = dd["lb_logits"]; lbsel_d = dd["lbsel"]; rope_d = dd["rope"]
        cst_d = dd["cst"]; resetm_d = dd["resetm"]; gdec_d = dd["gdec"]
        Rin_d = dd.get("Rin"); Sin_d = dd.get("Sin"); Lsl_d = dd.get("Lsl"); rcoef_d = dd.get("rcoef")
        w_ro_d = dd.get("w_ret_out"); w_ho_d = dd.get("w_hg_out"); w_o_d = dd.get("w_o"); gng_d = dd.get("gn_g_b")
        hgn_d = dd.get("hgn_g"); ln_d = dd.get("ln"); wr_d = dd.get("wr"); mg_d = dd.get("mg"); mu_d = dd.get("mu"); md_d = dd.get("md")
        wg_d = dd.get("wg"); wu_d = dd.get("wu"); wd_d = dd.get("wd"); hout_d = dd.get("hT_out"); oT_d = dd.get("oT_scr")
        h1_d = dd.get("h1_scr")
        Rloc_d = dd.get("Rloc"); Sloc_d = dd.get("Sloc"); Lsum_d = dd.get("Lsum"); Rst_d = dd.get("Rst"); Sst_d = dd.get("Sst")
        def sb(name, shape, dt=F32):
            key = "s_" + name
            if key not in cache:
                cache[key] = nc.alloc_sbuf_tensor(key, list(shape), dt)
            return cache[key]

        ARENA_W = 29184
        if "arena" not in cache:
            cache["arena"] = nc.alloc_sbuf_tensor("arena", [128, ARENA_W], F32)
        arena_t = cache["arena"]
        ast = {"off": 0}

        def arena_reset():
            P.barrier()
            ast["off"] = 0

        def ar(name, shape, dt=F32):
            n = 1
            for d_ in shape[1:]:
                n *= d_
            words = (n + 1) // 2 if dt == BF16 else n
            words = (words + 7) // 8 * 8
            off = ast["off"]
            assert off + words <= ARENA_W, (name, off, words)
            ast["off"] = off + words
            ap = arena_t[:, off:off + words]
            if dt == BF16:
                ap = ap.bitcast(BF16)
            ap = ap[:, 0:n]
            if len(shape) == 3:
                ap = ap.rearrange("p (a b) -> p a b", a=shape[1])
            return ap

        cst = sb("cst", [128, 6, 128])
        resetm = sb("resetm", [128, NT])
        gdec = sb("gdec", [128, 8, 2])
        identb = sb("identb", [128, 128], BF16)
        onesb = sb("onesb", [128, 128], BF16)
        lbl = sb("lbl", [128, 4, 16])
        lbsel = sb("lbsel", [128, 4])
        lbv = sb("lbv", [128, 16])
        omlv = sb("omlv", [128, 16])
        hTb = sb("hTb", [128, 16, NT], BF16)

        P.emit("sp", lambda e: e.dma_start(out=cst[:], in_=cst_d), writes=[Bf("cst")], dma_buf=Bf("cst"))
        P.emit("sp", lambda e: e.dma_start(out=resetm[:], in_=resetm_d), writes=[Bf("resetm")], dma_buf=Bf("resetm"))
        P.emit("sp", lambda e: e.dma_start(out=gdec[:], in_=gdec_d), writes=[Bf("gdec")], dma_buf=Bf("gdec"))
        P.emit("sp", lambda e: e.dma_start(out=lbl[:], in_=lbl_d), writes=[Bf("lbl")], dma_buf=Bf("lbl"))
        P.emit("sp", lambda e: e.dma_start(out=lbsel[:], in_=lbsel_d), writes=[Bf("lbsel")], dma_buf=Bf("lbsel"))
        P.emit("pool", lambda e: e.dma_start(out=identb[:], in_=cst_d[:, 2, :]), writes=[Bf("identb")], dma_buf=Bf("identb"))
        P.emit("pool", lambda e: e.dma_start(out=onesb[:], in_=cst_d[:, 4, :]), writes=[Bf("onesb")], dma_buf=Bf("onesb"))
        for kc4 in range(4):
            P.emit("pool", lambda e, kc4=kc4: e.dma_start(
                out=hTb[:, kc4 * 4:(kc4 + 1) * 4, :],
                in_=hT_d[kc4 * 512:(kc4 + 1) * 512, :].rearrange("(c p) t -> p c t", p=128)),
                writes=[Bf(f"hTb{kc4}")], dma_buf=Bf(f"hTb{kc4}"))
        HTB = [Bf(f"hTb{i}") for i in range(4)]
        causal = cst[:, 0, :]
        bdmask = cst[:, 1, :]
        cmask0 = cst[:, 3, :]
        onesf = cst[:, 4, :]
        ind = cst[:, 5, 0:4]
        rowmask = cst[:, 5, 4:5]
        CST = Bf("cst")

        lbe = sb("lbe", [128, 4, 16])
        lbs = sb("lbs", [128, 16])
        P.emit("act", lambda e: e.activation(out=lbe[:], in_=lbl[:], func=AF.Exp), reads=[Bf("lbl")], writes=[Bf("lbe")])
        P.emit("dve", lambda e: e.tensor_tensor(out=lbs[:], in0=lbe[:, 0, :], in1=lbe[:, 1, :], op=ALU.add),
               reads=[Bf("lbe")], writes=[Bf("lbs")])
        P.emit("dve", lambda e: e.tensor_tensor(out=lbs[:], in0=lbs[:], in1=lbe[:, 2, :], op=ALU.add),
               reads=[Bf("lbe"), Bf("lbs")], writes=[Bf("lbs")])
        P.emit("dve", lambda e: e.tensor_tensor(out=lbs[:], in0=lbs[:], in1=lbe[:, 3, :], op=ALU.add),
               reads=[Bf("lbe"), Bf("lbs")], writes=[Bf("lbs")])
        P.emit("dve", lambda e: e.reciprocal(out=lbs[:], in_=lbs[:]), reads=[Bf("lbs")], writes=[Bf("lbs")])
        P.emit("dve", lambda e: e.memset(lbv[:], 0.0), writes=[Bf("lbv")])
        for i in range(1, 4):
            P.emit("dve", lambda e, i=i: e.scalar_tensor_tensor(out=lbv[:], in0=lbe[:, i, :], scalar=lbsel[:, i:i + 1],
                                                                in1=lbv[:], op0=ALU.mult, op1=ALU.add),
                   reads=[Bf("lbe"), Bf("lbsel"), Bf("lbv")], writes=[Bf("lbv")])
        P.emit("dve", lambda e: e.tensor_tensor(out=lbv[:], in0=lbv[:], in1=lbs[:], op=ALU.mult),
               reads=[Bf("lbv"), Bf("lbs")], writes=[Bf("lbv")])
        P.emit("dve", lambda e: e.tensor_scalar(out=omlv[:], in0=lbv[:], scalar1=-1.0, scalar2=1.0, op0=ALU.mult, op1=ALU.add),
               reads=[Bf("lbv")], writes=[Bf("omlv")])
        LBV = [Bf("lbv"), Bf("omlv")]

        NW = 3
        wbufs = [sb(f"wbuf{i}", [128, 16, 512], BF16) for i in range(NW)]
        wstate = {"i": 0}

        def wload(dram2d, K, C):
            i = wstate["i"] % NW
            wstate["i"] += 1
            t = wbufs[i]
            b = Bf(f"wbuf{i}")
            kcn = K // 128
            P.emit("pool", lambda e: e.dma_start(out=t[:, 0:kcn, 0:C], in_=dram2d.rearrange("(c p) n -> p c n", p=128)),
                   writes=[b], dma_buf=b)
            return t, b, kcn

        def rsqrt_ops(out, in_, addc, rbufs, wbuf):
            P.emit("dve", lambda e: e.tensor_scalar(out=out, in0=in_, scalar1=addc, scalar2=None, op0=ALU.add),
                   reads=rbufs, writes=[wbuf])
            P.emit("act", lambda e: e.activation(out=out, in_=out, func=AF.Ln), reads=[wbuf], writes=[wbuf])
            P.emit("act", lambda e: e.activation(out=out, in_=out, func=AF.Exp, scale=-0.5), reads=[wbuf], writes=[wbuf])

        if "pb" not in cache:
            cache["pb"] = [nc.alloc_psum_tensor(f"pb{i}", [128, 512], F32) for i in range(7)]
            cache["ptr"] = nc.alloc_psum_tensor("ptr", [128, 1024], BF16)
        pb = cache["pb"]
        ptr = cache["ptr"]
        PB = [Bf(f"pb{i}") for i in range(7)]
        PTR = Bf("ptr")

        def proj_fm(wt, wb, c0, evac):
            for kc in range(16):
                for tb in range(3):
                    ps = pb[tb]
                    P.emit("pe", lambda e, ps=ps, kc=kc, tb=tb: e.matmul(
                        ps[:, 0:384], wt[:, kc, c0:c0 + 128], hTb[:, kc, tb * 384:(tb + 1) * 384],
                        start=(kc == 0), stop=(kc == 15)),
                        reads=[wb, HTB[kc // 4]], writes=[PB[tb]])
            for tb in range(3):
                evac(tb, pb[tb][:, 0:384], PB[tb])

        def proj_tm(wt, wb, C, evac):
            for i in range(NTILE):
                bank = 3 + (i % 2)
                ps = pb[bank]
                for kc in range(16):
                    P.emit("pe", lambda e, ps=ps, kc=kc, i=i: e.matmul(
                        ps[:, 0:C], hTb[:, kc, i * 128:(i + 1) * 128], wt[:, kc, 0:C],
                        start=(kc == 0), stop=(kc == 15)),
                        reads=[wb, HTB[kc // 4]], writes=[PB[bank]])
                evac(i, ps[:, 0:C], PB[bank])

        Lsum = sb("Lsum", [128, 16])
        if full:
            hgn = sb("hgn", [128, 16])
            hgs = sb("hgs", [128, 16])
            P.emit("sp", lambda e: e.dma_start(out=hgn[:], in_=hgn_d), writes=[Bf("hgn")], dma_buf=Bf("hgn"))
            P.emit("dve", lambda e: e.tensor_scalar(out=hgs[:], in0=hgn[:], scalar1=float(np.sqrt(128.0)), scalar2=None, op0=ALU.mult),
                   reads=[Bf("hgn")], writes=[Bf("hgs")])
            if stmode == "slots":
                rcoef = sb("rcoef", [128, 3, 8])
                P.emit("sp", lambda e: e.dma_start(out=rcoef[:], in_=rcoef_d), writes=[Bf("rcoef")], dma_buf=Bf("rcoef"))
                Lsl = sb("Lsl", [128, 3, 16])
                Fsl = sb("Fsl", [128, 3, 16])
                P.emit("sp", lambda e: e.dma_start(out=Lsl[:], in_=Lsl_d), writes=[Bf("Lsl")], dma_buf=Bf("Lsl"))
                P.emit("act", lambda e: e.activation(out=Fsl[:], in_=Lsl[:], func=AF.Exp), reads=[Bf("Lsl")], writes=[Bf("Fsl")])
            lnp = sb("lnp", [128, 4, 16])
            P.emit("sp", lambda e: e.dma_start(out=lnp[:], in_=ln_d), writes=[Bf("lnp")], dma_buf=Bf("lnp"))

        def phase_ret():
            x1 = ar("x1", [128, NT])
            x2 = ar("x2", [128, NT])
            rt = ar("rope_t", [128, 4, NT])
            QT = ar("QT", [128, 2, NT], BF16)
            KT = ar("KT", [128, 2, NT], BF16)
            Ktm = ar("Ktm", [128, NTILE, 256], BF16)
            V = ar("V", [128, NTILE, 512], BF16)
            T32 = ar("T32", [128, 2, 512])
            Rb = ar("Rb", [128, 2, 512], BF16)
            tmpa = ar("tmpa", [128, NT])
            tmpb = ar("tmpb", [128, NT])
            if full:
                G = ar("G", [128, NTILE, 512], BF16)
                AT = ar("AT", [128, 128], BF16)
                gngh = ar("gngh", [128, 512])
                st4 = ar("st4", [128, 8])
                osb = ar("osb", [128, 512])
                osb2 = ar("osb2", [128, 512])
                opb = ar("opb", [128, 512], BF16)
                oTs = ar("oTs", [128, 4, NT], BF16)
                junk = ar("junk", [128, 512])
                Rs = ar("Rs", [128, 3, 512])

            for h in range(_DBG.get('ret_heads', 8)):
                P.emit("sp", lambda e, h=h: e.dma_start(out=rt, in_=rope_d[h].rearrange("f p t -> p f t")),
                       writes=[Bf("rt")], dma_buf=Bf("rt"))
                RT = Bf("rt")

                def rot(which, dst, dstB):
                    c = rt[:, 2 * which, :]
                    s_ = rt[:, 2 * which + 1, :]
                    P.emit("dve", lambda e: e.tensor_tensor(out=tmpa, in0=x1, in1=c, op=ALU.mult),
                           reads=[Bf("x1"), RT], writes=[Bf("tmpa")])
                    P.emit("dve", lambda e: e.tensor_tensor(out=tmpb, in0=x2, in1=s_, op=ALU.mult),
                           reads=[Bf("x2"), RT], writes=[Bf("tmpb")])
                    P.emit("dve", lambda e: e.tensor_tensor(out=dst[:, 0, :], in0=tmpa, in1=tmpb, op=ALU.subtract),
                           reads=[Bf("tmpa"), Bf("tmpb")], writes=[dstB])
                    P.emit("dve", lambda e: e.tensor_tensor(out=tmpa, in0=x1, in1=s_, op=ALU.mult),
                           reads=[Bf("x1"), RT], writes=[Bf("tmpa")])
                    P.emit("dve", lambda e: e.tensor_tensor(out=tmpb, in0=x2, in1=c, op=ALU.mult),
                           reads=[Bf("x2"), RT], writes=[Bf("tmpb")])
                    P.emit("dve", lambda e: e.tensor_tensor(out=dst[:, 1, :], in0=tmpa, in1=tmpb, op=ALU.add),
                           reads=[Bf("tmpa"), Bf("tmpb")], writes=[dstB])

                def proj_rot(off, which, dst, dstB):
                    wt, wb, _ = wload(w_in_d[:, off + h * 256: off + (h + 1) * 256], D, 256)
                    for half, xx, xb in ((0, x1, Bf("x1")), (1, x2, Bf("x2"))):
                        def ev(tb, ps, psb, xx=xx, xb=xb):
                            P.emit("act", lambda e: e.activation(out=xx[:, tb * 384:(tb + 1) * 384], in_=ps, func=AF.Copy),
                                   reads=[psb], writes=[xb])
                        proj_fm(wt, wb, half * 128, ev)
                    rot(which, dst, dstB)

                if full:
                    proj_rot(OFF_RQ, 0, QT, Bf("QT"))
                proj_rot(OFF_RK, 1, KT, Bf("KT"))
                for i in range(NTILE):
                    for half in range(2):
                        P.emit("pe", lambda e, i=i, half=half: e.transpose(
                            ptr[:, half * 128:(half + 1) * 128], KT[:, half, i * 128:(i + 1) * 128], identb[:]),
                            reads=[Bf("KT"), Bf("identb")], writes=[PTR])
                    P.emit("act", lambda e, i=i: e.activation(out=Ktm[:, i, :], in_=ptr[:, 0:256], func=AF.Copy),
                           reads=[PTR], writes=[Bf(f"Ktm{i}")])
                wt, wb, _ = wload(w_in_d[:, OFF_RV + h * 512: OFF_RV + (h + 1) * 512], D, 512)

                def evV(i, ps, psb):
                    if i == 0:
                        P.emit("act", lambda e: e.activation(out=V[:, 0, :], in_=ps, func=AF.Copy, scale=rowmask),
                               reads=[psb, CST], writes=[Bf("V0")])
                    else:
                        P.emit("act", lambda e: e.activation(out=V[:, i, :], in_=ps, func=AF.Copy),
                               reads=[psb], writes=[Bf(f"V{i}")])
                proj_tm(wt, wb, 512, evV)
                if full:
                    wt, wb, _ = wload(w_in_d[:, OFF_RG + h * 512: OFF_RG + (h + 1) * 512], D, 512)

                    def evG(i, ps, psb):
                        P.emit("act", lambda e: e.activation(out=G[:, i, :], in_=ps, func=AF.Silu),
                               reads=[psb], writes=[Bf(f"G{i}")])
                    proj_tm(wt, wb, 512, evG)
                    P.emit("sp", lambda e, h=h: e.dma_start(out=gngh, in_=gng_d[:, h * 512:(h + 1) * 512]),
                           writes=[Bf("gngh")], dma_buf=Bf("gngh"))
                    if stmode == "slots":
                        for hf in range(2):
                            for s_ in range(3):
                                P.emit("sp", lambda e, s_=s_, hf=hf, h=h: e.dma_start(out=Rs[:, s_, :], in_=Rin_d[s_, h, hf]),
                                       writes=[Bf(f"Rs{s_}")], dma_buf=Bf(f"Rs{s_}"))
                            P.emit("dve", lambda e, hf=hf, h=h: e.scalar_tensor_tensor(
                                out=T32[:, hf, :], in0=Rs[:, 0, :], scalar=rcoef[:, 0, h:h + 1], in1=Rs[:, 1, :],
                                op0=ALU.mult, op1=ALU.add), reads=[Bf("Rs0"), Bf("Rs1"), Bf("rcoef")], writes=[Bf(f"T32_{hf}")])
                            P.emit("dve", lambda e, hf=hf, h=h: e.scalar_tensor_tensor(
                                out=T32[:, hf, :], in0=T32[:, hf, :], scalar=rcoef[:, 1, h:h + 1], in1=Rs[:, 2, :],
                                op0=ALU.mult, op1=ALU.add), reads=[Bf("Rs2"), Bf(f"T32_{hf}"), Bf("rcoef")], writes=[Bf(f"T32_{hf}")])
                            P.emit("dve", lambda e, hf=hf, h=h: e.tensor_scalar(
                                out=T32[:, hf, :], in0=T32[:, hf, :], scalar1=rcoef[:, 2, h:h + 1], scalar2=None, op0=ALU.mult),
                                reads=[Bf(f"T32_{hf}"), Bf("rcoef")], writes=[Bf(f"T32_{hf}")])
                            P.emit("act", lambda e, hf=hf: e.activation(out=Rb[:, hf, :], in_=T32[:, hf, :], func=AF.Copy),
                                   reads=[Bf(f"T32_{hf}")], writes=[Bf(f"Rb{hf}")])
                    elif seg == 0:
                        for hf in range(2):
                            P.emit("dve", lambda e, hf=hf: e.memset(T32[:, hf, :], 0.0), writes=[Bf(f"T32_{hf}")])
                            P.emit("act", lambda e, hf=hf: e.activation(out=Rb[:, hf, :], in_=T32[:, hf, :], func=AF.Copy),
                                   reads=[Bf(f"T32_{hf}")], writes=[Bf(f"Rb{hf}")])
                    else:
                        for hf in range(2):
                            P.emit("sp", lambda e, hf=hf, h=h: e.dma_start(out=T32[:, hf, :], in_=Rst_d[h, hf]),
                                   writes=[Bf(f"T32_{hf}")], dma_buf=Bf(f"T32ld_{hf}"))
                            P.emit("act", lambda e, hf=hf: e.activation(out=Rb[:, hf, :], in_=T32[:, hf, :], func=AF.Copy),
                                   reads=[Bf(f"T32_{hf}")], writes=[Bf(f"Rb{hf}")])
                else:
                    for hf in range(2):
                        P.emit("dve", lambda e, hf=hf: e.memset(T32[:, hf, :], 0.0), writes=[Bf(f"T32_{hf}")])

                for i in range(NTILE):
                    cols = slice(i * 128, (i + 1) * 128)
                    Vi = Bf(f"V{i}")
                    if full:
                        for hf in range(2):
                            P.emit("pe", lambda e, hf=hf, cols=cols: e.matmul(pb[5][:, 0:128], KT[:, hf, cols], QT[:, hf, cols],
                                                                              start=(hf == 0), stop=(hf == 1)),
                                   reads=[Bf("KT"), Bf("QT")], writes=[PB[5]])
                        P.emit("dve", lambda e: e.tensor_tensor(out=AT, in0=pb[5][:, 0:128], in1=causal, op=ALU.mult),
                               reads=[PB[5], CST], writes=[Bf("AT")])
                        P.emit("pe", lambda e, i=i: e.matmul(pb[6][:, :], AT, V[:, i, :], start=True, stop=False),
                               reads=[Bf("AT"), Vi], writes=[PB[6]])
                        for hf in range(2):
                            P.emit("pe", lambda e, hf=hf, cols=cols: e.matmul(pb[6][:, :], QT[:, hf, cols], Rb[:, hf, :],
                                                                              start=False, stop=(hf == 1)),
                                   reads=[Bf("QT"), Bf(f"Rb{hf}")], writes=[PB[6]])
                        P.emit("act", lambda e: e.activation(out=osb2, in_=pb[6][:, :], func=AF.Copy),
                               reads=[PB[6]], writes=[Bf("osb2")])
                        P.emit("dve", lambda e: e.reduce_sum(out=st4[:, 0:1], in_=osb2, axis=AX.X),
                               reads=[Bf("osb2")], writes=[Bf("st_sum")])
                        P.emit("act", lambda e: e.activation(out=junk, in_=osb2, func=AF.Square),
                               reads=[Bf("osb2")], writes=[Bf("junk")])
                        P.emit("dve", lambda e: e.reduce_sum(out=st4[:, 1:2], in_=junk, axis=AX.X),
                               reads=[Bf("junk")], writes=[Bf("st_sq")])
                        P.emit("dve", lambda e: e.tensor_scalar(out=st4[:, 2:3], in0=st4[:, 0:1], scalar1=1.0 / 512, scalar2=None, op0=ALU.mult),
                               reads=[Bf("st_sum")], writes=[Bf("st_mean")])
                        P.emit("dve", lambda e: e.tensor_tensor(out=st4[:, 3:4], in0=st4[:, 2:3], in1=st4[:, 2:3], op=ALU.mult),
                               reads=[Bf("st_mean")], writes=[Bf("st_m2")])
                        P.emit("dve", lambda e: e.scalar_tensor_tensor(out=st4[:, 4:5], in0=st4[:, 1:2], scalar=1.0 / 512, in1=st4[:, 3:4],
                                                                       op0=ALU.mult, op1=ALU.subtract),
                               reads=[Bf("st_sq"), Bf("st_m2")], writes=[Bf("st_var")])
                        rsqrt_ops(st4[:, 5:6], st4[:, 4:5], EPS, [Bf("st_var")], Bf("st_rstd"))
                        P.emit("dve", lambda e: e.tensor_scalar(out=osb, in0=osb2, scalar1=st4[:, 2:3], scalar2=st4[:, 5:6],
                                                                op0=ALU.subtract, op1=ALU.mult),
                               reads=[Bf("osb2"), Bf("st_mean"), Bf("st_rstd")], writes=[Bf("osb")])
                        P.emit("dve", lambda e: e.tensor_tensor(out=osb2, in0=osb, in1=gngh, op=ALU.mult),
                               reads=[Bf("osb"), Bf("gngh")], writes=[Bf("osb2")])
                        P.emit("dve", lambda e, i=i: e.tensor_tensor(out=opb, in0=osb2, in1=G[:, i, :], op=ALU.mult),
                               reads=[Bf("osb2"), Bf(f"G{i}")], writes=[Bf("opb")])
                        for c in range(4):
                            P.emit("pe", lambda e, c=c: e.transpose(ptr[:, (4 + c) * 128:(5 + c) * 128],
                                                                    opb[:, c * 128:(c + 1) * 128], identb[:]),
                                   reads=[Bf("opb"), Bf("identb")], writes=[PTR])
                        for c in range(4):
                            P.emit("act", lambda e, cols=cols, c=c: e.activation(
                                out=oTs[:, c, cols], in_=ptr[:, (4 + c) * 128:(5 + c) * 128], func=AF.Copy),
                                reads=[PTR], writes=[Bf("oTs")])
                    for hf in range(2):
                        P.emit("pe", lambda e, hf=hf, i=i: e.matmul(pb[4][:, :], Ktm[:, i, hf * 128:(hf + 1) * 128], V[:, i, :],
                                                                    start=True, stop=True),
                               reads=[Bf(f"Ktm{i}"), Vi], writes=[PB[4]])
                        if i == 0:
                            P.emit("dve", lambda e, hf=hf: e.tensor_tensor(out=T32[:, hf, :], in0=T32[:, hf, :], in1=pb[4][:, :], op=ALU.add),
                                   reads=[PB[4], Bf(f"T32_{hf}")], writes=[Bf(f"T32_{hf}")])
                        else:
                            dsc = gdec[:, h, 1:2] if i == 1 else gdec[:, h, 0:1]
                            P.emit("dve", lambda e, hf=hf, dsc=dsc: e.scalar_tensor_tensor(
                                out=T32[:, hf, :], in0=T32[:, hf, :], scalar=dsc, in1=pb[4][:, :], op0=ALU.mult, op1=ALU.add),
                                reads=[PB[4], Bf(f"T32_{hf}"), Bf("gdec")], writes=[Bf(f"T32_{hf}")])
                        dnext = gdec[:, h, 1:2] if i == 0 else gdec[:, h, 0:1]
                        if full and i < NTILE - 1:
                            P.emit("act", lambda e, hf=hf, dnext=dnext: e.activation(out=Rb[:, hf, :], in_=T32[:, hf, :], func=AF.Copy, scale=dnext),
                                   reads=[Bf(f"T32_{hf}"), Bf("gdec")], writes=[Bf(f"Rb{hf}")])
                        if stmode == "chain" and seg < 3 and i == NTILE - 1:
                            P.emit("act", lambda e, hf=hf, dnext=dnext: e.activation(out=T32[:, hf, :], in_=T32[:, hf, :], func=AF.Copy, scale=dnext),
                                   reads=[Bf(f"T32_{hf}"), Bf("gdec")], writes=[Bf(f"T32_{hf}")])
                            P.emit("sp", lambda e, hf=hf, h=h: e.dma_start(out=Rst_d[h, hf], in_=T32[:, hf, :]),
                                   reads=[Bf(f"T32_{hf}")], writes=[Bf("Rst_d")], dma_buf=Bf(f"T32st_{hf}"))
                        if (not full) and i == NTILE - 1:
                            P.emit("act", lambda e, hf=hf, dnext=dnext: e.activation(out=T32[:, hf, :], in_=T32[:, hf, :], func=AF.Copy, scale=dnext),
                                   reads=[Bf(f"T32_{hf}"), Bf("gdec")], writes=[Bf(f"T32_{hf}")])
                            final_ops.append(P.emit("sp", lambda e, hf=hf, h=h: e.dma_start(out=Rloc_d[h, hf], in_=T32[:, hf, :]),
                                                    reads=[Bf(f"T32_{hf}")], writes=[Bf("Rloc_d")], dma_buf=Bf(f"T32st_{hf}")))
                if full:
                    for c in range(4):
                        P.emit("sp", lambda e, c=c, h=h: e.dma_start(out=oT_d[h * 4 + c], in_=oTs[:, c, :]),
                               reads=[Bf("oTs")], writes=[Bf(f"oT_d{h * 4 + c}")], dma_buf=Bf(f"oTs_st{c}"))

        def phase_hg():
            sg = ar("sg", [128, NT])
            gl = ar("gl", [128, NT])
            bb = ar("bb", [128, NT])
            eb = ar("eb", [128, NT])
            enb = ar("enb", [128, NT])
            KhT = ar("KhT", [128, NT], BF16)
            V4 = ar("V4", [128, NTILE, 512], BF16)
            Kt4 = ar("Kt4", [128, 4, 128], BF16)
            Th = ar("Th", [128, 128])
            Sb = ar("Sb", [128, 128], BF16)
            if full:
                QhT = ar("QhT", [128, NT], BF16)
                GhT = ar("GhT", [128, NT], BF16)
                sq = ar("sq", [128, NT])
                ATh = ar("ATh", [128, 128], BF16)
                sqb = ar("sqb", [128, 128], BF16)
                rstd = ar("rstd", [128, 128])
                otmp = ar("otmp", [128, 128])
                ohT = ar("ohT", [128, NT], BF16)
                Ss = ar("Ss", [128, 3, 128])
            for gidx in range(_DBG.get('hg_groups', 4)):
                wi_t, wi_b, _ = wload(w_in_d[:, OFF_HI + gidx * 512: OFF_HI + (gidx + 1) * 512], D, 512)

                def evV(i, ps, psb):
                    if i == 0:
                        P.emit("act", lambda e: e.activation(out=V4[:, 0, :], in_=ps, func=AF.Copy, scale=rowmask),
                               reads=[psb, CST], writes=[Bf("V4_0")])
                    else:
                        P.emit("act", lambda e: e.activation(out=V4[:, i, :], in_=ps, func=AF.Copy),
                               reads=[psb], writes=[Bf(f"V4_{i}")])
                proj_tm(wi_t, wi_b, 512, evV)
                for hh in range(4):
                    H = gidx * 4 + hh
                    wf_t, wf_b, _ = wload(w_in_d[:, OFF_HF + H * 128: OFF_HF + (H + 1) * 128], D, 128)

                    def evF(tb, ps, psb):
                        P.emit("act", lambda e: e.activation(out=sg[:, tb * 384:(tb + 1) * 384], in_=ps, func=AF.Sigmoid),
                               reads=[psb], writes=[Bf("sg")])
                    proj_fm(wf_t, wf_b, 0, evF)
                    P.emit("dve", lambda e, H=H: e.tensor_scalar(out=sg, in0=sg, scalar1=omlv[:, H:H + 1], scalar2=lbv[:, H:H + 1],
                                                                 op0=ALU.mult, op1=ALU.add),
                           reads=[Bf("sg")] + LBV, writes=[Bf("sg")])
                    P.emit("act", lambda e: e.activation(out=gl, in_=sg, func=AF.Ln), reads=[Bf("sg")], writes=[Bf("gl")])
                    P.emit("dve", lambda e: e.tensor_tensor(out=gl[:, 0:128], in0=gl[:, 0:128], in1=cmask0, op=ALU.mult),
                           reads=[Bf("gl"), CST], writes=[Bf("gl")])
                    P.emit("dve", lambda e: e.tensor_tensor_scan(out=bb, data0=resetm[:], data1=gl, initial=0.0,
                                                                 op0=ALU.mult, op1=ALU.add),
                           reads=[Bf("gl"), Bf("resetm")], writes=[Bf("bb")])
                    P.emit("act", lambda e: e.activation(out=eb, in_=bb, func=AF.Exp), reads=[Bf("bb")], writes=[Bf("eb")])
                    P.emit("act", lambda e: e.activation(out=enb, in_=bb, func=AF.Exp, scale=-1.0), reads=[Bf("bb")], writes=[Bf("enb")])
                    P.emit("dve", lambda e: e.tensor_scalar(out=sg, in0=sg, scalar1=-1.0, scalar2=1.0, op0=ALU.mult, op1=ALU.add),
                           reads=[Bf("sg")], writes=[Bf("sg")])
                    P.emit("dve", lambda e: e.tensor_tensor(out=KhT, in0=sg, in1=enb, op=ALU.mult),
                           reads=[Bf("sg"), Bf("enb")], writes=[Bf("KhT")])
                    P.emit("dve", lambda e, H=H: e.reduce_sum(out=Lsum[:, H:H + 1], in_=bb[:, 31::32], axis=AX.X),
                           reads=[Bf("bb")], writes=[Bf("Lsum")])
                    if full:
                        wq_t, wq_b, _ = wload(w_in_d[:, OFF_HQ + H * 128: OFF_HQ + (H + 1) * 128], D, 128)

                        def evQ(tb, ps, psb):
                            P.emit("act", lambda e: e.activation(out=sq[:, tb * 384:(tb + 1) * 384], in_=ps, func=AF.Silu),
                                   reads=[psb], writes=[Bf("sq")])
                        proj_fm(wq_t, wq_b, 0, evQ)
                        P.emit("dve", lambda e: e.tensor_tensor(out=QhT, in0=sq, in1=eb, op=ALU.mult),
                               reads=[Bf("sq"), Bf("eb")], writes=[Bf("QhT")])
                        wg_t, wg_b, _ = wload(w_in_d[:, OFF_HG + H * 128: OFF_HG + (H + 1) * 128], D, 128)

                        def evG(tb, ps, psb):
                            P.emit("act", lambda e: e.activation(out=GhT[:, tb * 384:(tb + 1) * 384], in_=ps, func=AF.Silu),
                                   reads=[psb], writes=[Bf("GhT")])
                        proj_fm(wg_t, wg_b, 0, evG)
                        if stmode == "slots":
                            for s_ in range(3):
                                P.emit("sp", lambda e, s_=s_, H=H: e.dma_start(out=Ss[:, s_, :], in_=Sin_d[s_, H]),
                                       writes=[Bf(f"Ss{s_}")], dma_buf=Bf(f"Ss{s_}"))
                            P.emit("dve", lambda e, H=H: e.scalar_tensor_tensor(out=Th, in0=Ss[:, 0, :], scalar=Fsl[:, 1, H:H + 1], in1=Ss[:, 1, :],
                                                                                op0=ALU.mult, op1=ALU.add),
                                   reads=[Bf("Ss0"), Bf("Ss1"), Bf("Fsl")], writes=[Bf("Th")])
                            P.emit("dve", lambda e, H=H: e.scalar_tensor_tensor(out=Th, in0=Th, scalar=Fsl[:, 2, H:H + 1], in1=Ss[:, 2, :],
                                                                                op0=ALU.mult, op1=ALU.add),
                                   reads=[Bf("Th"), Bf("Ss2"), Bf("Fsl")], writes=[Bf("Th")])
                        elif seg == 0:
                            P.emit("dve", lambda e: e.memset(Th, 0.0), writes=[Bf("Th")])
                        else:
                            P.emit("sp", lambda e, H=H: e.dma_start(out=Th, in_=Sst_d[H]), writes=[Bf("Th")], dma_buf=Bf("Thld"))
                    else:
                        P.emit("dve", lambda e: e.memset(Th, 0.0), writes=[Bf("Th")])
                    Eprev = None
                    for i in range(NTILE):
                        cols = slice(i * 128, (i + 1) * 128)
                        Vi = Bf(f"V4_{i}")
                        vsl = V4[:, i, hh * 128:(hh + 1) * 128]
                        P.emit("pe", lambda e, cols=cols: e.transpose(ptr[:, 512:640], KhT[:, cols], identb[:]),
                               reads=[Bf("KhT"), Bf("identb")], writes=[PTR])
                        for j in range(4):
                            P.emit("act", lambda e, j=j: e.activation(out=Kt4[:, j, :], in_=ptr[:, 512:640], func=AF.Copy, scale=ind[:, j:j + 1]),
                                   reads=[PTR, CST], writes=[Bf(f"Kt4_{j}")])
                        for j in range(4):
                            P.emit("pe", lambda e, j=j, vsl=vsl: e.matmul(pb[4][:, j * 128:(j + 1) * 128], Kt4[:, j, :], vsl, start=True, stop=True),
                                   reads=[Bf(f"Kt4_{j}"), Vi], writes=[PB[4]])
                        if full:
                            P.emit("pe", lambda e, cols=cols: e.matmul(pb[5][:, 0:128], KhT[:, cols], QhT[:, cols], start=True, stop=True),
                                   reads=[Bf("KhT"), Bf("QhT")], writes=[PB[5]])
                            P.emit("dve", lambda e: e.tensor_tensor(out=ATh, in0=pb[5][:, 0:128], in1=bdmask, op=ALU.mult),
                                   reads=[PB[5], CST], writes=[Bf("ATh")])
                            P.emit("pe", lambda e, vsl=vsl: e.matmul(pb[6][:, 0:128], vsl, ATh, start=True, stop=False),
                                   reads=[Vi, Bf("ATh")], writes=[PB[6]])
                        for j in range(4):
                            c0 = i * 128 + j * 32
                            if full:
                                if Eprev is None:
                                    P.emit("act", lambda e: e.activation(out=Sb, in_=Th, func=AF.Copy), reads=[Bf("Th")], writes=[Bf("Sb")])
                                else:
                                    P.emit("act", lambda e, Eprev=Eprev: e.activation(out=Sb, in_=Th, func=AF.Copy, scale=Eprev),
                                           reads=[Bf("Th"), Bf("eb")], writes=[Bf("Sb")])
                                P.emit("pe", lambda e, j=j, c0=c0: e.matmul(pb[6][:, j * 32:(j + 1) * 32], Sb, QhT[:, c0:c0 + 32],
                                                                            start=False, stop=(j == 3)),
                                       reads=[Bf("Sb"), Bf("QhT")], writes=[PB[6]])
                            if Eprev is None:
                                P.emit("dve", lambda e, j=j: e.tensor_tensor(out=Th, in0=Th, in1=pb[4][:, j * 128:(j + 1) * 128], op=ALU.add),
                                       reads=[PB[4], Bf("Th")], writes=[Bf("Th")])
                            else:
                                P.emit("dve", lambda e, j=j, Eprev=Eprev: e.scalar_tensor_tensor(
                                    out=Th, in0=Th, scalar=Eprev, in1=pb[4][:, j * 128:(j + 1) * 128], op0=ALU.mult, op1=ALU.add),
                                    reads=[PB[4], Bf("Th"), Bf("eb")], writes=[Bf("Th")])
                            Eprev = eb[:, c0 + 31:c0 + 32]
                        if full:
                            P.emit("act", lambda e: e.activation(out=sqb, in_=pb[6][:, 0:128], func=AF.Square), reads=[PB[6]], writes=[Bf("sqb")])
                            P.emit("pe", lambda e: e.matmul(pb[5][:, 128:256], onesb[:], sqb, start=True, stop=True),
                                   reads=[Bf("sqb"), Bf("onesb")], writes=[PB[5]])
                            rsqrt_ops(rstd, pb[5][:, 128:256], 128.0 * EPS, [PB[5]], Bf("rstd"))
                            P.emit("dve", lambda e: e.tensor_tensor(out=otmp, in0=pb[6][:, 0:128], in1=rstd, op=ALU.mult),
                                   reads=[PB[6], Bf("rstd")], writes=[Bf("otmp")])
                            P.emit("dve", lambda e, H=H, cols=cols: e.scalar_tensor_tensor(
                                out=ohT[:, cols], in0=otmp, scalar=hgs[:, H:H + 1], in1=GhT[:, cols], op0=ALU.mult, op1=ALU.mult),
                                reads=[Bf("otmp"), Bf("hgs"), Bf("GhT")], writes=[Bf("ohT")])
                    if full:
                        P.emit("sp", lambda e, H=H: e.dma_start(out=oT_d[32 + H], in_=ohT),
                               reads=[Bf("ohT")], writes=[Bf(f"oT_d{32 + H}")], dma_buf=Bf("ohT_st"))
                        if stmode == "chain" and seg < 3:
                            P.emit("act", lambda e, Eprev=Eprev: e.activation(out=Th, in_=Th, func=AF.Copy, scale=Eprev),
                                   reads=[Bf("Th"), Bf("eb")], writes=[Bf("Th")])
                            P.emit("sp", lambda e, H=H: e.dma_start(out=Sst_d[H], in_=Th),
                                   reads=[Bf("Th")], writes=[Bf("Sst_d")], dma_buf=Bf("Th_st"))
                    else:
                        P.emit("act", lambda e, Eprev=Eprev: e.activation(out=Th, in_=Th, func=AF.Copy, scale=Eprev),
                               reads=[Bf("Th"), Bf("eb")], writes=[Bf("Th")])
                        final_ops.append(P.emit("sp", lambda e, H=H: e.dma_start(out=Sloc_d[H], in_=Th),
                                                reads=[Bf("Th")], writes=[Bf("Sloc_d")], dma_buf=Bf("Th_st")))

        if "mix" in phases:
            phase_ret()
            arena_reset()
            phase_hg()
            if not full:
                final_ops.append(P.emit("sp", lambda e: e.dma_start(out=Lsum_d, in_=Lsum[:]),
                                        reads=[Bf("Lsum")], writes=[Bf("Lsum_d")], dma_buf=Bf("Lsum_st")))

        def acc_mm(bankpair, units, ncol0, rhs_fn, rhs_bufs):
            total = sum(u[2] for u in units)
            k = 0
            for (wt, wb, kcn) in units:
                for kc in range(kcn):
                    for hb in range(2):
                        P.emit("pe", lambda e, wt=wt, kc=kc, k=k, hb=hb: e.matmul(
                            pb[bankpair + hb][:, 0:HB], wt[:, kc, ncol0:ncol0 + 128], rhs_fn(k, hb),
                            start=(k == 0), stop=(k == total - 1)),
                            reads=[wb] + rhs_bufs, writes=[PB[bankpair + hb]])
                    k += 1

        def phase_tok(tb):
            t0 = tb * TB
            tcols = slice(t0, t0 + TB)
            mg_ = ar("merged", [128, 16, TB], BF16)
            z = ar("z", [128, 16, TB])
            mark = ast["off"]
            orT = ar("orT", [128, 32, TB], BF16)
            ohT2 = ar("ohT2", [128, 16, TB], BF16)
            sga = ar("sga", [128, TB])
            t1 = ar("t1", [128, TB])
            for c4 in range(8):
                P.emit("sp", lambda e, c4=c4: e.dma_start(out=orT[:, c4 * 4:(c4 + 1) * 4, :],
                                                          in_=oT_d[c4 * 4:(c4 + 1) * 4, :, tcols].rearrange("c p t -> p c t")),
                       reads=[Bf(f"oT_d{c4 * 4 + i}") for i in range(4)], writes=[Bf(f"orT{c4}")], dma_buf=Bf(f"orT{c4}"))
            for c4 in range(4):
                P.emit("sp", lambda e, c4=c4: e.dma_start(out=ohT2[:, c4 * 4:(c4 + 1) * 4, :],
                                                          in_=oT_d[32 + c4 * 4:32 + (c4 + 1) * 4, :, tcols].rearrange("c p t -> p c t")),
                       reads=[Bf(f"oT_d{32 + c4 * 4 + i}") for i in range(4)], writes=[Bf(f"ohT2{c4}")], dma_buf=Bf(f"ohT2{c4}"))
            ORT = [Bf(f"orT{c}") for c in range(8)]
            OHT = [Bf(f"ohT2{c}") for c in range(4)]
            for ng in range(4):
                ncs = slice(ng * 512, (ng + 1) * 512)
                u_ro = [wload(w_ro_d[0:2048, ncs], 2048, 512), wload(w_ro_d[2048:4096, ncs], 2048, 512)]
                u_ga = [wload(w_in_d[:, OFF_GA + ng * 512: OFF_GA + (ng + 1) * 512], D, 512)]
                for nn in range(4):
                    n = ng * 4 + nn
                    acc_mm(0, u_ro, nn * 128, lambda k, hb: orT[:, k, hb * HB:(hb + 1) * HB], ORT)
                    acc_mm(2, u_ga, nn * 128, lambda k, hb: hTb[:, k, t0 + hb * HB: t0 + (hb + 1) * HB], HTB)
                    for hb in range(2):
                        P.emit("act", lambda e, hb=hb: e.activation(out=sga[:, hb * HB:(hb + 1) * HB], in_=pb[2 + hb][:, 0:HB], func=AF.Sigmoid),
                               reads=[PB[2 + hb]], writes=[Bf("sga")])
                        P.emit("dve", lambda e, hb=hb, n=n: e.tensor_tensor(out=z[:, n, hb * HB:(hb + 1) * HB], in0=pb[hb][:, 0:HB],
                                                                            in1=sga[:, hb * HB:(hb + 1) * HB], op=ALU.mult),
                               reads=[PB[hb], Bf("sga")], writes=[Bf(f"z{n}")])
            for ng in range(4):
                ncs = slice(ng * 512, (ng + 1) * 512)
                u_ho = [wload(w_ho_d[:, ncs], D, 512)]
                u_gb = [wload(w_in_d[:, OFF_GB + ng * 512: OFF_GB + (ng + 1) * 512], D, 512)]
                for nn in range(4):
                    n = ng * 4 + nn
                    acc_mm(0, u_ho, nn * 128, lambda k, hb: ohT2[:, k, hb * HB:(hb + 1) * HB], OHT)
                    acc_mm(2, u_gb, nn * 128, lambda k, hb: hTb[:, k, t0 + hb * HB: t0 + (hb + 1) * HB], HTB)
                    for hb in range(2):
                        P.emit("act", lambda e, hb=hb: e.activation(out=sga[:, hb * HB:(hb + 1) * HB], in_=pb[2 + hb][:, 0:HB], func=AF.Sigmoid),
                               reads=[PB[2 + hb]], writes=[Bf("sga")])
                        P.emit("dve", lambda e, hb=hb: e.tensor_tensor(out=t1[:, hb * HB:(hb + 1) * HB], in0=pb[hb][:, 0:HB],
                                                                       in1=sga[:, hb * HB:(hb + 1) * HB], op=ALU.mult),
                               reads=[PB[hb], Bf("sga")], writes=[Bf("t1")])
                    P.emit("dve", lambda e, n=n: e.tensor_tensor(out=mg_[:, n, :], in0=z[:, n, :], in1=t1, op=ALU.add),
                           reads=[Bf("t1"), Bf(f"z{n}")], writes=[Bf(f"mg{n}")])
            P.barrier()
            ast["off"] = mark
            zsq = ar("zsq", [128, TB])
            mean = ar("mean", [128, TB])
            rs = ar("rs", [128, TB])
            nmr = ar("nmr", [128, TB])
            MG = [Bf(f"mg{n}") for n in range(16)]
            for n4 in range(4):
                P.emit("sp", lambda e, n4=n4: e.dma_start(out=z[:, n4 * 4:(n4 + 1) * 4, :],
                                                          in_=hT_d[n4 * 512:(n4 + 1) * 512, tcols].rearrange("(c p) t -> p c t", p=128)),
                       writes=[Bf(f"z{n4 * 4 + i}") for i in range(4)], dma_buf=Bf(f"zld{n4}"))
            for ng in range(4):
                u_o = [wload(w_o_d[:, ng * 512:(ng + 1) * 512], D, 512)]
                for nn in range(4):
                    n = ng * 4 + nn
                    acc_mm(0, u_o, nn * 128, lambda k, hb: mg_[:, k, hb * HB:(hb + 1) * HB], MG)
                    for hb in range(2):
                        P.emit("dve", lambda e, n=n, hb=hb: e.scalar_tensor_tensor(
                            out=z[:, n, hb * HB:(hb + 1) * HB], in0=z[:, n, hb * HB:(hb + 1) * HB], scalar=ALPHA, in1=pb[hb][:, 0:HB],
                            op0=ALU.mult, op1=ALU.add), reads=[PB[hb], Bf(f"z{n}")], writes=[Bf(f"z{n}")])

            def after1(n):
                P.emit("act", lambda e, n=n: e.activation(out=hTb[:, n, tcols], in_=z[:, n, :], func=AF.Copy),
                       reads=[Bf(f"z{n}")], writes=[HTB[n // 4]])
                P.emit("sp", lambda e, n=n: e.dma_start(out=h1_d[n * 128:(n + 1) * 128, tcols], in_=z[:, n, :]),
                       reads=[Bf(f"z{n}")], writes=[Bf(f"h1d{n}")], dma_buf=Bf(f"zst{n}"))
            layer_norm(lambda n, cs: z[:, n, cs], lambda n: Bf(f"z{n}"), zsq, mean, rs, nmr, 0, 1, after1)

        def layer_norm(zf, zb, zsq, mean, rs, nmr, gi, bi, after):
            for hb in range(2):
                cs = slice(hb * HB, (hb + 1) * HB)
                for n in range(16):
                    P.emit("pe", lambda e, n=n, cs=cs: e.matmul(pb[4][:, 0:HB], onesf, zf(n, cs), start=(n == 0), stop=(n == 15)),
                           reads=[zb(n), CST], writes=[PB[4]])
                for n in range(16):
                    P.emit("act", lambda e, n=n, cs=cs: e.activation(out=zsq[:, 0:HB], in_=zf(n, cs), func=AF.Square),
                           reads=[zb(n)], writes=[Bf("zsq")])
                    P.emit("pe", lambda e, n=n: e.matmul(pb[5][:, 0:HB], onesf, zsq[:, 0:HB], start=(n == 0), stop=(n == 15)),
                           reads=[Bf("zsq"), CST], writes=[PB[5]])
                P.emit("dve", lambda e, cs=cs: e.tensor_scalar(out=mean[:, cs], in0=pb[4][:, 0:HB], scalar1=1.0 / D, scalar2=None, op0=ALU.mult),
                       reads=[PB[4]], writes=[Bf("mean")])
                P.emit("dve", lambda e, cs=cs: e.tensor_tensor(out=nmr[:, cs], in0=mean[:, cs], in1=mean[:, cs], op=ALU.mult),
                       reads=[Bf("mean")], writes=[Bf("nmr")])
                P.emit("dve", lambda e, cs=cs: e.scalar_tensor_tensor(out=rs[:, cs], in0=pb[5][:, 0:HB], scalar=1.0 / D, in1=nmr[:, cs],
                                                                      op0=ALU.mult, op1=ALU.subtract),
                       reads=[PB[5], Bf("nmr")], writes=[Bf("rs")])
                rsqrt_ops(rs[:, cs], rs[:, cs], EPS, [Bf("rs")], Bf("rs"))
            allc = slice(0, TB)
            for n in range(16):
                P.emit("dve", lambda e, n=n: e.tensor_tensor(out=zf(n, allc), in0=zf(n, allc), in1=mean, op=ALU.subtract),
                       reads=[zb(n), Bf("mean")], writes=[zb(n)])
                P.emit("dve", lambda e, n=n: e.tensor_tensor(out=zf(n, allc), in0=zf(n, allc), in1=rs, op=ALU.mult),
                       reads=[zb(n), Bf("rs")], writes=[zb(n)])
                P.emit("dve", lambda e, n=n: e.tensor_scalar(out=zf(n, allc), in0=zf(n, allc), scalar1=lnp[:, gi, n:n + 1], scalar2=lnp[:, bi, n:n + 1],
                                                             op0=ALU.mult, op1=ALU.add),
                       reads=[zb(n), Bf("lnp")], writes=[zb(n)])
                after(n)

        def phase_ffn():
            zA = ar("zA", [128, 16, NT])
            act = ar("actT", [128, 4, NT], BF16)
            t1 = ar("t1b", [128, TB])
            t2 = ar("t2b", [128, TB])
            zsq = ar("zsq", [128, TB])
            mean = ar("mean", [128, TB])
            rs = ar("rs", [128, TB])
            nmr = ar("nmr", [128, TB])
            ZA = [Bf(f"zA{n}") for n in range(16)]
            for n4 in range(4):
                P.emit("sp", lambda e, n4=n4: e.dma_start(out=zA[:, n4 * 4:(n4 + 1) * 4, :],
                                                          in_=h1_d[n4 * 512:(n4 + 1) * 512, :].rearrange("(c p) t -> p c t", p=128)),
                       reads=[Bf(f"h1d{n4 * 4 + i}") for i in range(4)], writes=[ZA[n4 * 4 + i] for i in range(4)], dma_buf=Bf(f"zAld{n4}"))
            for n in range(16):
                P.emit("act", lambda e, n=n: e.activation(out=zA[:, n, :], in_=zA[:, n, :], func=AF.Copy, scale=ALPHA),
                       reads=[ZA[n]], writes=[ZA[n]])

            def ffn(wg_ap, wu_ap, wd_ap, F, gate=None):
                nchunks = F // 128
                c = 0
                while c < nchunks:
                    gsz = min(4, nchunks - c)
                    fc0 = c * 128
                    u_g = [wload(wg_ap[:, fc0:fc0 + gsz * 128], D, gsz * 128)]
                    u_u = [wload(wu_ap[:, fc0:fc0 + gsz * 128], D, gsz * 128)]
                    for j in range(gsz):
                        for pr in range(NTB):
                            p0 = pr * TB
                            acc_mm(0, u_g, j * 128, lambda k, hb, p0=p0: hTb[:, k, p0 + hb * HB:p0 + (hb + 1) * HB], HTB)
                            for hb in range(2):
                                P.emit("act", lambda e, hb=hb: e.activation(out=t1[:, hb * HB:(hb + 1) * HB], in_=pb[hb][:, 0:HB], func=AF.Silu),
                                       reads=[PB[hb]], writes=[Bf("t1")])
                            acc_mm(2, u_u, j * 128, lambda k, hb, p0=p0: hTb[:, k, p0 + hb * HB:p0 + (hb + 1) * HB], HTB)
                            for hb in range(2):
                                dsl = slice(p0 + hb * HB, p0 + (hb + 1) * HB)
                                if gate is None:
                                    P.emit("dve", lambda e, hb=hb, j=j, dsl=dsl: e.tensor_tensor(
                                        out=act[:, j, dsl], in0=t1[:, hb * HB:(hb + 1) * HB], in1=pb[2 + hb][:, 0:HB], op=ALU.mult),
                                        reads=[PB[2 + hb], Bf("t1")], writes=[Bf(f"act{j}")])
                                else:
                                    P.emit("dve", lambda e, hb=hb: e.tensor_tensor(
                                        out=t2[:, hb * HB:(hb + 1) * HB], in0=t1[:, hb * HB:(hb + 1) * HB], in1=pb[2 + hb][:, 0:HB], op=ALU.mult),
                                        reads=[PB[2 + hb], Bf("t1")], writes=[Bf("t2")])
                                    P.emit("dve", lambda e, hb=hb, j=j, dsl=dsl: e.tensor_tensor(
                                        out=act[:, j, dsl], in0=t2[:, hb * HB:(hb + 1) * HB], in1=gate[:, dsl], op=ALU.mult),
                                        reads=[Bf("t2"), Bf("gbe")], writes=[Bf(f"act{j}")])
                    ACT = [Bf(f"act{a}") for a in range(gsz)]
                    for ng in range(4):
                        u_d = [wload(wd_ap[c * 128:(c + gsz) * 128, ng * 512:(ng + 1) * 512], gsz * 128, 512)]
                        for nn in range(4):
                            n = ng * 4 + nn
                            for pr in range(NTB):
                                p0 = pr * TB
                                acc_mm(0, u_d, nn * 128, lambda k, hb, p0=p0: act[:, k, p0 + hb * HB:p0 + (hb + 1) * HB], ACT)
                                for hb in range(2):
                                    dsl = slice(p0 + hb * HB, p0 + (hb + 1) * HB)
                                    P.emit("dve", lambda e, n=n, hb=hb, dsl=dsl: e.tensor_tensor(
                                        out=zA[:, n, dsl], in0=zA[:, n, dsl], in1=pb[hb][:, 0:HB], op=ALU.add),
                                        reads=[PB[hb], ZA[n]], writes=[ZA[n]])
                    c += gsz

            if not moe:
                ffn(wg_d, wu_d, wd_d, DFF)
            else:
                NG = NT // 96
                wrf = ar("wrf", [128, 16, NE])
                P.emit("sp", lambda e: e.dma_start(out=wrf, in_=wr_d.rearrange("(c p) n -> p c n", p=128)),
                       writes=[Bf("wrf")], dma_buf=Bf("wrf"))
                lg = ar("lg", [128, 16])
                gts = ar("gts", [128, 4, NE])
                gtA = ar("gtA", [128, NG, NE])
                gexp = ar("gexp", [128, 128])
                gbe = ar("gbe", [128, NT], BF16)
                identf = cst[:, 2, :]
                for gq in range(NG):
                    ts_ = slice(gq * 96, (gq + 1) * 96)
                    for kc in range(16):
                        P.emit("pe", lambda e, kc=kc, ts_=ts_: e.matmul(pb[4][0:96, 0:NE], zA[:, kc, ts_], wrf[:, kc, :],
                                                                        start=(kc == 0), stop=(kc == 15)),
                               reads=[ZA[kc], Bf("wrf")], writes=[PB[4]])
                    L = gts[0:96, 0, :]
                    P.emit("act", lambda e, L=L: e.activation(out=L, in_=pb[4][0:96, 0:NE], func=AF.Copy, scale=1.0 / ALPHA),
                           reads=[PB[4]], writes=[Bf("gts0")])
                    m1 = lg[0:96, 0:1]
                    m2 = lg[0:96, 1:2]
                    P.emit("dve", lambda e, L=L, m1=m1: e.reduce_max(out=m1, in_=L, axis=AX.X), reads=[Bf("gts0")], writes=[Bf("lg_m1")])
                    E1 = gts[0:96, 1, :]
                    P.emit("dve", lambda e, L=L, m1=m1, E1=E1: e.tensor_scalar(out=E1, in0=L, scalar1=m1, scalar2=None, op0=ALU.is_equal),
                           reads=[Bf("gts0"), Bf("lg_m1")], writes=[Bf("gts1")])
                    L2 = gts[0:96, 2, :]
                    P.emit("dve", lambda e, L=L, E1=E1, L2=L2: e.scalar_tensor_tensor(out=L2, in0=E1, scalar=-1e30, in1=L, op0=ALU.mult, op1=ALU.add),
                           reads=[Bf("gts0"), Bf("gts1")], writes=[Bf("gts2")])
                    P.emit("dve", lambda e, L2=L2, m2=m2: e.reduce_max(out=m2, in_=L2, axis=AX.X), reads=[Bf("gts2")], writes=[Bf("lg_m2")])
                    E2 = gts[0:96, 3, :]
                    P.emit("dve", lambda e, L2=L2, m2=m2, E2=E2: e.tensor_scalar(out=E2, in0=L2, scalar1=m2, scalar2=None, op0=ALU.is_equal),
                           reads=[Bf("gts2"), Bf("lg_m2")], writes=[Bf("gts3")])
                    dd_ = lg[0:96, 2:3]
                    w1 = lg[0:96, 3:4]
                    w2 = lg[0:96, 4:5]
                    P.emit("dve", lambda e, m1=m1, m2=m2, dd_=dd_: e.tensor_tensor(out=dd_, in0=m2, in1=m1, op=ALU.subtract),
                           reads=[Bf("lg_m1"), Bf("lg_m2")], writes=[Bf("lg_dd")])
                    P.emit("act", lambda e, dd_=dd_: e.activation(out=dd_, in_=dd_, func=AF.Exp), reads=[Bf("lg_dd")], writes=[Bf("lg_dd")])
                    P.emit("dve", lambda e, dd_=dd_: e.tensor_scalar(out=dd_, in0=dd_, scalar1=1.0, scalar2=None, op0=ALU.add),
                           reads=[Bf("lg_dd")], writes=[Bf("lg_dd")])
                    P.emit("dve", lambda e, dd_=dd_, w1=w1: e.reciprocal(out=w1, in_=dd_), reads=[Bf("lg_dd")], writes=[Bf("lg_w1")])
                    P.emit("dve", lambda e, w1=w1, w2=w2: e.tensor_scalar(out=w2, in0=w1, scalar1=-1.0, scalar2=1.0, op0=ALU.mult, op1=ALU.add),
                           reads=[Bf("lg_w1")], writes=[Bf("lg_w2")])
                    Gt = gtA[0:96, gq, :]
                    P.emit("dve", lambda e, E1=E1, w1=w1, Gt=Gt: e.tensor_scalar(out=Gt, in0=E1, scalar1=w1, scalar2=None, op0=ALU.mult),
                           reads=[Bf("gts1"), Bf("lg_w1")], writes=[Bf("gtA")])
                    P.emit("dve", lambda e, E2=E2, w2=w2, Gt=Gt: e.scalar_tensor_tensor(out=Gt, in0=E2, scalar=w2, in1=Gt, op0=ALU.mult, op1=ALU.add),
                           reads=[Bf("gts3"), Bf("lg_w2"), Bf("gtA")], writes=[Bf("gtA")])
                for ee in range(NE):
                    for gq in range(NG):
                        ts_ = slice(gq * 96, (gq + 1) * 96)
                        P.emit("dve", lambda e, ee=ee, gq=gq: e.tensor_scalar(out=gexp[0:96, :], in0=onesf[0:96, :], scalar1=gtA[0:96, gq, ee:ee + 1],
                                                                             scalar2=None, op0=ALU.mult),
                               reads=[Bf("gtA"), CST], writes=[Bf("gexp")])
                        P.emit("pe", lambda e: e.matmul(pb[5][:, 0:96], gexp[0:96, :], identf[0:96, 0:96], start=True, stop=True),
                               reads=[Bf("gexp"), CST], writes=[PB[5]])
                        P.emit("act", lambda e, ts_=ts_: e.activation(out=gbe[:, ts_], in_=pb[5][:, 0:96], func=AF.Copy),
                               reads=[PB[5]], writes=[Bf("gbe")])
                    ffn(mg_d[ee], mu_d[ee], md_d[ee], DFE, gate=gbe)

            for pr in range(NTB):
                p0 = pr * TB

                def store(n, p0=p0):
                    final_ops.append(P.emit("sp", lambda e, n=n: e.dma_start(out=hout_d[n * 128:(n + 1) * 128, p0:p0 + TB], in_=zA[:, n, p0:p0 + TB]),
                                            reads=[ZA[n]], writes=[Bf("hout")], dma_buf=Bf(f"zst{n}")))
                layer_norm(lambda n, cs, p0=p0: zA[:, n, p0 + cs.start:p0 + cs.stop], lambda n: ZA[n], zsq, mean, rs, nmr, 2, 3, store)

        if full and "tok" in phases:
            for tb in range(NTB):
                arena_reset()
                phase_tok(tb)
            arena_reset()
            phase_ffn()

    if kind in ("A", "B"):
        full_ = kind == "B"
        dd = dict(hT=din("hT", [D, NT]), w_in=din("w_in", [D, P_IN]), lb_logits=din("lb_logits", [128, 4, 16]),
                  lbsel=din("lbsel", [128, 4]), rope=din("rope", [8, 4, 128, NT]), cst=din("cst", [128, 6, 128]),
                  resetm=din("resetm", [128, NT]), gdec=din("gdec", [128, 8, 2]))
        if full_:
            dd.update(Rin=din("Rin", [3, 8, 2, 128, 512]), Sin=din("Sin", [3, 16, 128, 128]), Lsl=din("Lsl", [128, 3, 16]),
                      rcoef=din("rcoef", [128, 3, 8]), w_ret_out=din("w_ret_out", [4096, D]), w_hg_out=din("w_hg_out", [D, D]),
                      w_o=din("w_o", [D, D]), gn_g_b=din("gn_g_b", [128, 4096]), hgn_g=din("hgn_g", [128, 16]), ln=din("ln", [128, 4, 16]))
            if moe:
                dd.update(wr=din("wr", [D, NE]), mg=din("mg", [NE, D, DFE]), mu=din("mu", [NE, D, DFE]), md=din("md", [NE, DFE, D]))
            else:
                dd.update(wg=din("wg", [D, DFF]), wu=din("wu", [D, DFF]), wd=din("wd", [DFF, D]))
            dd.update(hT_out=dout("hT_out", [D, NT]), oT_scr=dscr("oT_scr", [48, 128, NT], BF16), h1_scr=dscr("h1_scr", [D, NT]))
        else:
            dd.update(Rloc=dout("Rloc", [8, 2, 128, 512]), Sloc=dout("Sloc", [16, 128, 128]), Lsum=dout("Lsum", [128, 16]))
        body(full_, moe, dd, "slots" if full_ else "zero", 0)
        P.finalize(final_wait_ops=final_ops)
        return nc

    NL = _DBG.get("nl", 4)
    NS = _DBG.get("ns", 4)
    hT_all = din("hT", [4, D, NT])
    w_in_all = din("w_in", [4, D, P_IN])
    lbl_d = din("lb_logits", [128, 4, 16])
    lbsel_all = din("lbsel", [4, 128, 4])
    rope_all = din("rope", [4, 8, 4, 128, NT])
    cst_all = din("cst", [4, 128, 6, 128])
    resetm_d = din("resetm", [128, NT])
    gdec_all = din("gdec", [4, 128, 8, 2])
    w_ro_all = din("w_ret_out", [4, 4096, D])
    w_ho_all = din("w_hg_out", [4, D, D])
    w_o_all = din("w_o", [4, D, D])
    gng_all = din("gn_g_b", [4, 128, 4096])
    hgn_all = din("hgn_g", [4, 128, 16])
    ln_all = din("ln", [4, 128, 4, 16])
    wr_all = din("wr", [2, D, NE])
    mg_all = din("mg", [2, NE, D, DFE])
    mu_all = din("mu", [2, NE, D, DFE])
    md_all = din("md", [2, NE, DFE, D])
    wg_all = din("wg", [2, D, DFF])
    wu_all = din("wu", [2, D, DFF])
    wd_all = din("wd", [2, DFF, D])
    hout_all = dout("hT_out", [4, D, NT])
    hbuf = dscr("hbuf", [2, 4, D, NT])
    oT_scr = dscr("oT_scr", [48, 128, NT], BF16)
    h1_scr = dscr("h1_scr", [D, NT])
    Rst = dscr("Rst", [4, 8, 2, 128, 512])
    Sst = dscr("Sst", [4, 16, 128, 128])
    for seg in range(NS):
        for l in range(NL):
            moe_l = (l % 2 == 1)
            dd = dict(hT=(hT_all[seg] if l == 0 else hbuf[(l - 1) % 2, seg]), w_in=w_in_all[l], lb_logits=lbl_d,
                      lbsel=lbsel_all[l], rope=rope_all[seg], cst=cst_all[seg], resetm=resetm_d, gdec=gdec_all[seg],
                      w_ret_out=w_ro_all[l], w_hg_out=w_ho_all[l], w_o=w_o_all[l], gn_g_b=gng_all[l], hgn_g=hgn_all[l],
                      ln=ln_all[l], hT_out=(hout_all[seg] if l == NL - 1 else hbuf[l % 2, seg]), oT_scr=oT_scr, h1_scr=h1_scr,
                      Rst=Rst[l], Sst=Sst[l])
            if moe_l:
                dd.update(wr=wr_all[l // 2], mg=mg_all[l // 2], mu=mu_all[l // 2], md=md_all[l // 2])
            else:
                dd.update(wg=wg_all[l // 2], wu=wu_all[l // 2], wd=wd_all[l // 2])
            P.barrier()
            body(True, moe_l, dd, "chain", seg)
    P.barrier()
    P.finalize(final_wait_ops=[])
    return nc


_PROGS = {}


def _prog(kind, moe=False):
    key = (kind, moe)
    if key not in _PROGS:
        _PROGS[key] = build(kind, moe)
    return _PROGS[key]


def _consts(rank):
    s = np.arange(128)
    causal = (s[:, None] <= s[None, :]).astype(np.float32)
    bd = causal * ((s[:, None] // 32) == (s[None, :] // 32))
    ident = np.eye(128, dtype=np.float32)
    cm0 = np.zeros((128, 128), np.float32)
    if rank == 0:
        cm0[:, 112:] = 1.0
    ones = np.ones((128, 128), np.float32)
    pk = np.zeros((128, 128), np.float32)
    for j in range(4):
        pk[j * 32:(j + 1) * 32, j] = 1.0
    if rank == 0:
        pk[112:, 4] = 1.0
    cst = np.ascontiguousarray(np.stack([causal, bd.astype(np.float32), ident, cm0, ones, pk], axis=1))
    resetm = np.ones((128, NT), np.float32)
    resetm[:, 0::32] = 0.0
    lg = np.log1p(-np.exp2(-5.0 - np.arange(8, dtype=np.float64)))
    gdec = np.zeros((128, 8, 2), np.float32)
    gdec[:, :, 0] = np.exp(128 * lg)[None, :]
    gdec[:, :, 1] = np.exp(128 * lg)[None, :] if rank == 0 else 1.0
    pos = np.zeros(NT, np.float64)
    if rank == 0:
        pos[112:128] = np.arange(16)
    pos[128:] = 16 + 1024 * rank + np.arange(1024)
    inv = 1.0 / (10000.0 ** np.linspace(0.0, 1.0, 128))
    ang = inv[:, None] * pos[None, :]
    cos, sin = np.cos(ang), np.sin(ang)
    j = (np.arange(NT) % 128).astype(np.float64)
    rope = np.zeros((8, 4, 128, NT), np.float32)
    for h in range(8):
        qd = np.exp((j + 1) * lg[h]) * (256.0 ** -0.5)
        kd = np.exp(-(j + 1) * lg[h])
        rope[h, 0] = cos * qd[None, :]
        rope[h, 1] = sin * qd[None, :]
        rope[h, 2] = cos * kd[None, :]
        rope[h, 3] = sin * kd[None, :]
    rc = np.ones((128, 3, 8), np.float32)
    g1024 = np.exp(1024 * lg).astype(np.float32)
    if rank == 2:
        rc[:, 0, :] = g1024[None, :]
    if rank == 3:
        rc[:, 0, :] = g1024[None, :]
        rc[:, 1, :] = g1024[None, :]
    return dict(cst=cst, resetm=resetm, gdec=gdec, rope=rope, rcoef=rc)


def _fm(v):
    return np.ascontiguousarray(v.reshape(-1, 128).T)


def kernel_fused(x, meta_tokens, w_in, ret_gn_g, hg_norm_g, hg_lb_logits, w_ret_out, w_hg_out, w_o,
                 ln1_g, ln1_b, ln2_g, ln2_b, ffn_w_gate, ffn_w_up, ffn_w_down,
                 moe_router, moe_w_gate, moe_w_up, moe_w_down, _cores=(0, 1)):
    f32 = lambda a: np.ascontiguousarray(np.asarray(a, dtype=np.float32))
    x = f32(x)
    NL = _DBG.get("nl", 4)
    consts = [_consts(r) for r in range(4)]
    shared = dict(
        w_in=f32(w_in), lb_logits=np.ascontiguousarray(f32(hg_lb_logits).reshape(4, 16, 128).transpose(2, 0, 1)),
        lbsel=np.ascontiguousarray(np.stack([np.concatenate([np.zeros((128, 1), np.float32),
                                                             np.broadcast_to((np.arange(1, 4) <= l).astype(np.float32)[None, :], (128, 3))], axis=1)
                                             for l in range(4)])),
        rope=np.ascontiguousarray(np.stack([c["rope"] for c in consts])),
        cst=np.ascontiguousarray(np.stack([c["cst"] for c in consts])),
        resetm=consts[0]["resetm"],
        gdec=np.ascontiguousarray(np.stack([c["gdec"] for c in consts])),
        w_ret_out=f32(w_ret_out), w_hg_out=f32(w_hg_out), w_o=f32(w_o),
        gn_g_b=np.ascontiguousarray(np.broadcast_to(f32(ret_gn_g)[:, None, :], (4, 128, 4096))),
        hgn_g=np.ascontiguousarray(np.stack([_fm(f32(hg_norm_g[l])) for l in range(4)])),
        ln=np.ascontiguousarray(np.stack([np.stack([_fm(f32(ln1_g[l])), _fm(f32(ln1_b[l])), _fm(f32(ln2_g[l])), _fm(f32(ln2_b[l]))], axis=1)
                                          for l in range(4)])),
        wr=f32(moe_router), mg=f32(moe_w_gate), mu=f32(moe_w_up), md=f32(moe_w_down),
        wg=f32(ffn_w_gate), wu=f32(ffn_w_up), wd=f32(ffn_w_down))
    in_maps = []
    for ci in _cores:
        b = ci % 2
        hT = np.zeros((4, D, NT), np.float32)
        hT[0, :, 112:128] = f32(meta_tokens).T
        for r in range(4):
            hT[r, :, 128:] = x[b, 1024 * r:1024 * (r + 1)].T
        in_maps.append(dict(shared, hT=hT))
    ncF = _prog("F")
    res = run_bass_kernel_spmd(ncF, in_maps, core_ids=list(range(len(_cores)))).results
    out = np.zeros((2, 4096, D), np.float32)
    for i, ci in enumerate(_cores[:2]):
        b = ci % 2
        ho = np.asarray(res[i]["hT_out"], dtype=np.float32)
        for r in range(4):
            out[b, 1024 * r:1024 * (r + 1)] = ho[r][:, 128:].T
    return out


def kernel_unfused(x, meta_tokens, w_in, ret_gn_g, hg_norm_g, hg_lb_logits, w_ret_out, w_hg_out, w_o,
           ln1_g, ln1_b, ln2_g, ln2_b, ffn_w_gate, ffn_w_up, ffn_w_down,
           moe_router, moe_w_gate, moe_w_up, moe_w_down, _nlayers=4, _debug=None):
    f32 = lambda a: np.ascontiguousarray(np.asarray(a, dtype=np.float32))
    x = f32(x)
    ncore = 8
    cores = [(c // 4, c % 4) for c in range(ncore)]
    consts = [_consts(r) for (_, r) in cores]
    hT = []
    for (b, r) in cores:
        t = np.zeros((D, NT), np.float32)
        if r == 0:
            t[:, 112:128] = f32(meta_tokens).T
        t[:, 128:] = x[b, 1024 * r:1024 * (r + 1)].T
        hT.append(t)
    lbl = np.ascontiguousarray(f32(hg_lb_logits).reshape(4, 16, 128).transpose(2, 0, 1))
    for l in range(_nlayers):
        moe = (l % 2 == 1)
        lbsel = np.zeros((128, 4), np.float32)
        lbsel[:, 1:l + 1] = 1.0
        w_in_l = f32(w_in[l])
        common = dict(w_in=w_in_l, lb_logits=lbl, lbsel=lbsel)
        ncA = _prog("A")
        in_maps = []
        for ci in range(ncore):
            cs = consts[ci]
            in_maps.append(dict(common, hT=hT[ci], rope=cs["rope"], cst=cs["cst"], resetm=cs["resetm"], gdec=cs["gdec"]))
        resA = run_bass_kernel_spmd(ncA, in_maps, core_ids=list(range(ncore))).results
        Bin = dict(common,
                   w_ret_out=f32(w_ret_out[l]), w_hg_out=f32(w_hg_out[l]), w_o=f32(w_o[l]),
                   gn_g_b=np.ascontiguousarray(np.broadcast_to(f32(ret_gn_g[l])[None, :], (128, 4096))),
                   hgn_g=_fm(f32(hg_norm_g[l])),
                   ln=np.ascontiguousarray(np.stack([_fm(f32(ln1_g[l])), _fm(f32(ln1_b[l])),
                                                     _fm(f32(ln2_g[l])), _fm(f32(ln2_b[l]))], axis=1)))
        if moe:
            Bin.update(wr=f32(moe_router[l // 2]), mg=f32(moe_w_gate[l // 2]), mu=f32(moe_w_up[l // 2]), md=f32(moe_w_down[l // 2]))
        else:
            Bin.update(wg=f32(ffn_w_gate[l // 2]), wu=f32(ffn_w_up[l // 2]), wd=f32(ffn_w_down[l // 2]))
        in_maps = []
        for ci, (b, r) in enumerate(cores):
            cs = consts[ci]
            Rin = np.zeros((3, 8, 2, 128, 512), np.float32)
            Sin = np.zeros((3, 16, 128, 128), np.float32)
            Lsl = np.zeros((128, 3, 16), np.float32)
            for jr in range(3):
                if jr < r:
                    src = resA[b * 4 + jr]
                    Rin[jr] = src["Rloc"]
                    Sin[jr] = src["Sloc"]
                    if jr >= 1:
                        Lsl[:, jr, :] = src["Lsum"]
            in_maps.append(dict(Bin, hT=hT[ci], rope=cs["rope"], cst=cs["cst"], resetm=cs["resetm"], gdec=cs["gdec"],
                                rcoef=cs["rcoef"], Rin=Rin, Sin=Sin, Lsl=Lsl))
        ncB = _prog("B", moe)
        resB = run_bass_kernel_spmd(ncB, in_maps, core_ids=list(range(ncore))).results
        hT = [np.asarray(resB[ci]["hT_out"], dtype=np.float32) for ci in range(ncore)]
        if _debug is not None:
            _debug.append([h.copy() for h in hT])
    out = np.zeros((2, 4096, D), np.float32)
    for ci, (b, r) in enumerate(cores):
        out[b, 1024 * r:1024 * (r + 1)] = hT[ci][:, 128:].T
    return out


FUSED = False


def kernel(**inputs):
    if FUSED:
        return kernel_fused(**inputs)
    return kernel_unfused(**inputs)
```

```python
import numpy as np
import concourse.bass as bass
import concourse.mybir as mybir
from concourse.bass_utils import run_bass_kernel_spmd

F32 = mybir.dt.float32
BF16 = mybir.dt.bfloat16
AF = mybir.ActivationFunctionType
ALU = mybir.AluOpType
AX = mybir.AxisListType

D = 2048
NT = 1152
NTILE = 9
P_IN = 24576
DFF = 5632
DFE = 2816
NE = 8
ALPHA = (2 * 4) ** 0.25
EPS = 1e-5
OFF_RQ, OFF_RK, OFF_RV, OFF_RG = 0, 2048, 4096, 8192
OFF_HQ, OFF_HF, OFF_HI, OFF_HG = 12288, 14336, 16384, 18432
OFF_GA, OFF_GB = 20480, 22528
TB = 576
NTB = 2
HB = 288


_DBG = {}


class Buf:
    def __init__(self, name):
        self.name = name
        self.w = None
        self.r = []
        self.sem = None
        self.semval = 0


class Op:
    __slots__ = ("eng", "fn", "deps", "dma_buf", "needed", "tok")

    def __init__(self, eng, fn, dma_buf=None):
        self.eng = eng
        self.fn = fn
        self.deps = []
        self.dma_buf = dma_buf
        self.needed = False
        self.tok = None


class Prog:
    ENGS = ("pe", "act", "dve", "pool", "sp")
    SEG = 30000

    def __init__(self, nc):
        self.nc = nc
        self.ops = {e: [] for e in self.ENGS}
        self.all_ops = []
        self.bufs = {}
        self.dma_since = []

    def B(self, name):
        b = self.bufs.get(name)
        if b is None:
            b = Buf(name)
            self.bufs[name] = b
        return b

    def emit(self, eng, fn, reads=(), writes=(), dma_buf=None):
        op = Op(eng, fn, dma_buf)
        deps = op.deps
        for b in reads:
            if b.w is not None:
                deps.append(b.w)
        for b in writes:
            if b.w is not None:
                deps.append(b.w)
            deps.extend(b.r)
        for b in reads:
            b.r.append(op)
        for b in writes:
            b.w = op
            b.r = []
        self.ops[eng].append(op)
        self.all_ops.append(op)
        if dma_buf is not None:
            self.dma_since.append(op)
        return op

    def barrier(self):
        deps = []
        for e in self.ENGS:
            for op in reversed(self.ops[e]):
                if op.fn is not None and op.dma_buf is None:
                    deps.append(op)
                    break
        deps.extend(self.dma_since)
        self.dma_since = []
        for e in self.ENGS:
            m = Op(e, None)
            m.deps = list(deps)
            self.ops[e].append(m)
            self.all_ops.append(m)
        for b in self.bufs.values():
            b.w = None
            b.r = []

    def finalize(self, final_wait_ops=()):
        nc = self.nc
        for op in self.all_ops:
            for d in op.deps:
                if d.dma_buf is not None or d.eng != op.eng or op.eng != "pe":
                    d.needed = True
        for op in final_wait_ops:
            op.needed = True
        eng_sems = {e: [] for e in self.ENGS}
        for e in self.ENGS:
            cnt = 0
            for op in self.ops[e]:
                if op.dma_buf is not None:
                    b = op.dma_buf
                    if b.sem is None:
                        b.sem = nc.alloc_semaphore("ds_" + b.name)
                    b.semval += 16
                    op.tok = (b.sem, b.semval, 16)
                elif op.needed:
                    seg = cnt // self.SEG
                    if seg >= len(eng_sems[e]):
                        eng_sems[e].append(nc.alloc_semaphore(f"es_{e}_{seg}"))
                    op.tok = (eng_sems[e][seg], cnt % self.SEG + 1, 1)
                    cnt += 1
        prog = self

        def run_engine(e, engine, extra_final=()):
            known = {}
            for op in prog.ops[e]:
                waits = {}
                for d in op.deps:
                    if d.tok is None:
                        continue
                    if d.dma_buf is None and d.eng == e and e == "pe":
                        continue
                    sem, val, _ = d.tok
                    key = id(sem)
                    if known.get(key, 0) >= val:
                        continue
                    if key not in waits or waits[key][1] < val:
                        waits[key] = (sem, val)
                for key, (sem, val) in waits.items():
                    engine.wait_ge(sem, val)
                    known[key] = val
                if op.fn is None:
                    continue
                ins = op.fn(engine)
                if op.tok is not None:
                    ins.then_inc(op.tok[0], op.tok[2])
            for d in extra_final:
                sem, val, _ = d.tok
                engine.wait_ge(sem, val)

        with nc.Block() as block:
            @block.tensor
            def _(eng):
                run_engine("pe", eng)

            @block.scalar
            def _(eng):
                run_engine("act", eng)

            @block.vector
            def _(eng):
                run_engine("dve", eng)

            @block.gpsimd
            def _(eng):
                run_engine("pool", eng)

            @block.sync
            def _(eng):
                run_engine("sp", eng, extra_final=final_wait_ops)


def build(kind, moe=False, phases=("mix", "tok")):
    nc = bass.Bass("TRN2", target_bir_lowering=False)
    P = Prog(nc)
    Bf = P.B
    cache = {}
    final_ops = []

    def din(name, shape, dt=F32):
        return nc.dram_tensor(name, list(shape), dt, kind="ExternalInput").ap()

    def dout(name, shape, dt=F32):
        return nc.dram_tensor(name, list(shape), dt, kind="ExternalOutput").ap()

    def dscr(name, shape, dt=F32):
        return nc.dram_tensor(name, list(shape), dt, kind="Internal").ap()

    def body(full, moe, dd, stmode, seg):
        hT_d = dd["hT"]; w_in_d = dd["w_in"]; lbl_d = dd["lb_logits"]; lbsel_d = dd["lbsel"]; rope_d = dd["rope"]
        cst_d = dd["cst"]; resetm_d = dd["resetm"]; gdec_d = dd["gdec"]
        Rin_d = dd.get("Rin"); Sin_d = dd.get("Sin"); Lsl_d = dd.get("Lsl"); rcoef_d = dd.get("rcoef")
        w_ro_d = dd.get("w_ret_out"); w_ho_d = dd.get("w_hg_out"); w_o_d = dd.get("w_o"); gng_d = dd.get("gn_g_b")
        hgn_d = dd.get("hgn_g"); ln_d = dd.get("ln"); wr_d = dd.get("wr"); mg_d = dd.get("mg"); mu_d = dd.get("mu"); md_d = dd.get("md")
        wg_d = dd.get("wg"); wu_d = dd.get("wu"); wd_d = dd.get("wd"); hout_d = dd.get("hT_out"); oT_d = dd.get("oT_scr")
        h1_d = dd.get("h1_scr")
        Rloc_d = dd.get("Rloc"); Sloc_d = dd.get("Sloc"); Lsum_d = dd.get("Lsum"); Rst_d = dd.get("Rst"); Sst_d = dd.get("Sst")
        def sb(name, shape, dt=F32):
            key = "s_" + name
            if key not in cache:
                cache[key] = nc.alloc_sbuf_tensor(key, list(shape), dt)
            return cache[key]

        ARENA_W = 29184
        if "arena" not in cache:
            cache["arena"] = nc.alloc_sbuf_tensor("arena", [128, ARENA_W], F32)
        arena_t = cache["arena"]
        ast = {"off": 0}

        def arena_reset():
            P.barrier()
            ast["off"] = 0

        def ar(name, shape, dt=F32):
            n = 1
            for d_ in shape[1:]:
                n *= d_
            words = (n + 1) // 2 if dt == BF16 else n
            words = (words + 7) // 8 * 8
            off = ast["off"]
            assert off + words <= ARENA_W, (name, off, words)
            ast["off"] = off + words
            ap = arena_t[:, off:off + words]
            if dt == BF16:
                ap = ap.bitcast(BF16)
            ap = ap[:, 0:n]
            if len(shape) == 3:
                ap = ap.rearrange("p (a b) -> p a b", a=shape[1])
            return ap

        cst = sb("cst", [128, 6, 128])
        resetm = sb("resetm", [128, NT])
        gdec = sb("gdec", [128, 8, 2])
        identb = sb("identb", [128, 128], BF16)
        onesb = sb("onesb", [128, 128], BF16)
        lbl = sb("lbl", [128, 4, 16])
        lbsel = sb("lbsel", [128, 4])
        lbv = sb("lbv", [128, 16])
        omlv = sb("omlv", [128, 16])
        hTb = sb("hTb", [128, 16, NT], BF16)

        P.emit("sp", lambda e: e.dma_start(out=cst[:], in_=cst_d), writes=[Bf("cst")], dma_buf=Bf("cst"))
        P.emit("sp", lambda e: e.dma_start(out=resetm[:], in_=resetm_d), writes=[Bf("resetm")], dma_buf=Bf("resetm"))
        P.emit("sp", lambda e: e.dma_start(out=gdec[:], in_=gdec_d), writes=[Bf("gdec")], dma_buf=Bf("gdec"))
        P.emit("sp", lambda e: e.dma_start(out=lbl[:], in_=lbl_d), writes=[Bf("lbl")], dma_buf=Bf("lbl"))
        P.emit("sp", lambda e: e.dma_start(out=lbsel[:], in_=lbsel_d), writes=[Bf("lbsel")], dma_buf=Bf("lbsel"))
        P.emit("pool", lambda e: e.dma_start(out=identb[:], in_=cst_d[:, 2, :]), writes=[Bf("identb")], dma_buf=Bf("identb"))
        P.emit("pool", lambda e: e.dma_start(out=onesb[:], in_=cst_d[:, 4, :]), writes=[Bf("onesb")], dma_buf=Bf("onesb"))
        for kc4 in range(4):
            P.emit("pool", lambda e, kc4=kc4: e.dma_start(
                out=hTb[:, kc4 * 4:(kc4 + 1) * 4, :],
                in_=hT_d[kc4 * 512:(kc4 + 1) * 512, :].rearrange("(c p) t -> p c t", p=128)),
                writes=[Bf(f"hTb{kc4}")], dma_buf=Bf(f"hTb{kc4}"))
        HTB = [Bf(f"hTb{i}") for i in range(4)]
        causal = cst[:, 0, :]
        bdmask = cst[:, 1, :]
        cmask0 = cst[:, 3, :]
        onesf = cst[:, 4, :]
        ind = cst[:, 5, 0:4]
        rowmask = cst[:, 5, 4:5]
        CST = Bf("cst")

        lbe = sb("lbe", [128, 4, 16])
        lbs = sb("lbs", [128, 16])
        P.emit("act", lambda e: e.activation(out=lbe[:], in_=lbl[:], func=AF.Exp), reads=[Bf("lbl")], writes=[Bf("lbe")])
        P.emit("dve", lambda e: e.tensor_tensor(out=lbs[:], in0=lbe[:, 0, :], in1=lbe[:, 1, :], op=ALU.add),
               reads=[Bf("lbe")], writes=[Bf("lbs")])
        P.emit("dve", lambda e: e.tensor_tensor(out=lbs[:], in0=lbs[:], in1=lbe[:, 2, :], op=ALU.add),
               reads=[Bf("lbe"), Bf("lbs")], writes=[Bf("lbs")])
        P.emit("dve", lambda e: e.tensor_tensor(out=lbs[:], in0=lbs[:], in1=lbe[:, 3, :], op=ALU.add),
               reads=[Bf("lbe"), Bf("lbs")], writes=[Bf("lbs")])
        P.emit("dve", lambda e: e.reciprocal(out=lbs[:], in_=lbs[:]), reads=[Bf("lbs")], writes=[Bf("lbs")])
        P.emit("dve", lambda e: e.memset(lbv[:], 0.0), writes=[Bf("lbv")])
        for i in range(1, 4):
            P.emit("dve", lambda e, i=i: e.scalar_tensor_tensor(out=lbv[:], in0=lbe[:, i, :], scalar=lbsel[:, i:i + 1],
                                                                in1=lbv[:], op0=ALU.mult, op1=ALU.add),
                   reads=[Bf("lbe"), Bf("lbsel"), Bf("lbv")], writes=[Bf("lbv")])
        P.emit("dve", lambda e: e.tensor_tensor(out=lbv[:], in0=lbv[:], in1=lbs[:], op=ALU.mult),
               reads=[Bf("lbv"), Bf("lbs")], writes=[Bf("lbv")])
        P.emit("dve", lambda e: e.tensor_scalar(out=omlv[:], in0=lbv[:], scalar1=-1.0, scalar2=1.0, op0=ALU.mult, op1=ALU.add),
               reads=[Bf("lbv")], writes=[Bf("omlv")])
        LBV = [Bf("lbv"), Bf("omlv")]

        NW = 3
        wbufs = [sb(f"wbuf{i}", [128, 16, 512], BF16) for i in range(NW)]
        wstate = {"i": 0}

        def wload(dram2d, K, C):
            i = wstate["i"] % NW
            wstate["i"] += 1
            t = wbufs[i]
            b = Bf(f"wbuf{i}")
            kcn = K // 128
            P.emit("pool", lambda e: e.dma_start(out=t[:, 0:kcn, 0:C], in_=dram2d.rearrange("(c p) n -> p c n", p=128)),
                   writes=[b], dma_buf=b)
            return t, b, kcn

        def rsqrt_ops(out, in_, addc, rbufs, wbuf):
            P.emit("dve", lambda e: e.tensor_scalar(out=out, in0=in_, scalar1=addc, scalar2=None, op0=ALU.add),
                   reads=rbufs, writes=[wbuf])
            P.emit("act", lambda e: e.activation(out=out, in_=out, func=AF.Ln), reads=[wbuf], writes=[wbuf])
            P.emit("act", lambda e: e.activation(out=out, in_=out, func=AF.Exp, scale=-0.5), reads=[wbuf], writes=[wbuf])

        if "pb" not in cache:
            cache["pb"] = [nc.alloc_psum_tensor(f"pb{i}", [128, 512], F32) for i in range(7)]
            cache["ptr"] = nc.alloc_psum_tensor("ptr", [128, 1024], BF16)
        pb = cache["pb"]
        ptr = cache["ptr"]
        PB = [Bf(f"pb{i}") for i in range(7)]
        PTR = Bf("ptr")

        def proj_fm(wt, wb, c0, evac):
            for tb in range(3):
                ps = pb[tb]
                for kc in range(16):
                    P.emit("pe", lambda e, ps=ps, kc=kc, tb=tb: e.matmul(
                        ps[:, 0:384], wt[:, kc, c0:c0 + 128], hTb[:, kc, tb * 384:(tb + 1) * 384],
                        start=(kc == 0), stop=(kc == 15)),
                        reads=[wb, HTB[kc // 4]], writes=[PB[tb]])
                evac(tb, ps[:, 0:384], PB[tb])

        def proj_tm(wt, wb, C, evac):
            for i in range(NTILE):
                bank = 3 + (i % 2)
                ps = pb[bank]
                for kc in range(16):
                    P.emit("pe", lambda e, ps=ps, kc=kc, i=i: e.matmul(
                        ps[:, 0:C], hTb[:, kc, i * 128:(i + 1) * 128], wt[:, kc, 0:C],
                        start=(kc == 0), stop=(kc == 15)),
                        reads=[wb, HTB[kc // 4]], writes=[PB[bank]])
                evac(i, ps[:, 0:C], PB[bank])

        Lsum = sb("Lsum", [128, 16])
        if full:
            hgn = sb("hgn", [128, 16])
            hgs = sb("hgs", [128, 16])
            P.emit("sp", lambda e: e.dma_start(out=hgn[:], in_=hgn_d), writes=[Bf("hgn")], dma_buf=Bf("hgn"))
            P.emit("dve", lambda e: e.tensor_scalar(out=hgs[:], in0=hgn[:], scalar1=float(np.sqrt(128.0)), scalar2=None, op0=ALU.mult),
                   reads=[Bf("hgn")], writes=[Bf("hgs")])
            if stmode == "slots":
                rcoef = sb("rcoef", [128, 3, 8])
                P.emit("sp", lambda e: e.dma_start(out=rcoef[:], in_=rcoef_d), writes=[Bf("rcoef")], dma_buf=Bf("rcoef"))
                Lsl = sb("Lsl", [128, 3, 16])
                Fsl = sb("Fsl", [128, 3, 16])
                P.emit("sp", lambda e: e.dma_start(out=Lsl[:], in_=Lsl_d), writes=[Bf("Lsl")], dma_buf=Bf("Lsl"))
                P.emit("act", lambda e: e.activation(out=Fsl[:], in_=Lsl[:], func=AF.Exp), reads=[Bf("Lsl")], writes=[Bf("Fsl")])
            lnp = sb("lnp", [128, 4, 16])
            P.emit("sp", lambda e: e.dma_start(out=lnp[:], in_=ln_d), writes=[Bf("lnp")], dma_buf=Bf("lnp"))

        def phase_ret():
            x1 = ar("x1", [128, NT])
            x2 = ar("x2", [128, NT])
            rt = ar("rope_t", [128, 4, NT])
            QT = ar("QT", [128, 2, NT], BF16)
            KT = ar("KT", [128, 2, NT], BF16)
            Ktm = ar("Ktm", [128, NTILE, 256], BF16)
            V = ar("V", [128, NTILE, 512], BF16)
            T32 = ar("T32", [128, 2, 512])
            Rb = ar("Rb", [128, 2, 512], BF16)
            tmpa = ar("tmpa", [128, NT])
            tmpb = ar("tmpb", [128, NT])
            if full:
                G = ar("G", [128, NTILE, 512], BF16)
                AT = ar("AT", [128, 128], BF16)
                gngh = ar("gngh", [128, 512])
                st4 = ar("st4", [128, 8])
                osb = ar("osb", [128, 512])
                osb2 = ar("osb2", [128, 512])
                opb = ar("opb", [128, 512], BF16)
                oTs = ar("oTs", [128, 4, NT], BF16)
                junk = ar("junk", [128, 512])
                Rs = ar("Rs", [128, 3, 512])

            for h in range(_DBG.get('ret_heads', 8)):
                P.emit("sp", lambda e, h=h: e.dma_start(out=rt, in_=rope_d[h].rearrange("f p t -> p f t")),
                       writes=[Bf("rt")], dma_buf=Bf("rt"))
                RT = Bf("rt")

                def rot(which, dst, dstB):
                    c = rt[:, 2 * which, :]
                    s_ = rt[:, 2 * which + 1, :]
                    P.emit("dve", lambda e: e.tensor_tensor(out=tmpa, in0=x1, in1=c, op=ALU.mult),
                           reads=[Bf("x1"), RT], writes=[Bf("tmpa")])
                    P.emit("dve", lambda e: e.tensor_tensor(out=tmpb, in0=x2, in1=s_, op=ALU.mult),
                           reads=[Bf("x2"), RT], writes=[Bf("tmpb")])
                    P.emit("dve", lambda e: e.tensor_tensor(out=dst[:, 0, :], in0=tmpa, in1=tmpb, op=ALU.subtract),
                           reads=[Bf("tmpa"), Bf("tmpb")], writes=[dstB])
                    P.emit("dve", lambda e: e.tensor_tensor(out=tmpa, in0=x1, in1=s_, op=ALU.mult),
                           reads=[Bf("x1"), RT], writes=[Bf("tmpa")])
                    P.emit("dve", lambda e: e.tensor_tensor(out=tmpb, in0=x2, in1=c, op=ALU.mult),
                           reads=[Bf("x2"), RT], writes=[Bf("tmpb")])
                    P.emit("dve", lambda e: e.tensor_tensor(out=dst[:, 1, :], in0=tmpa, in1=tmpb, op=ALU.add),
                           reads=[Bf("tmpa"), Bf("tmpb")], writes=[dstB])

                def proj_rot(off, which, dst, dstB):
                    wt, wb, _ = wload(w_in_d[:, off + h * 256: off + (h + 1) * 256], D, 256)
                    for half, xx, xb in ((0, x1, Bf("x1")), (1, x2, Bf("x2"))):
                        def ev(tb, ps, psb, xx=xx, xb=xb):
                            P.emit("act", lambda e: e.activation(out=xx[:, tb * 384:(tb + 1) * 384], in_=ps, func=AF.Copy),
                                   reads=[psb], writes=[xb])
                        proj_fm(wt, wb, half * 128, ev)
                    rot(which, dst, dstB)

                if full:
                    proj_rot(OFF_RQ, 0, QT, Bf("QT"))
                proj_rot(OFF_RK, 1, KT, Bf("KT"))
                for i in range(NTILE):
                    for half in range(2):
                        P.emit("pe", lambda e, i=i, half=half: e.transpose(
                            ptr[:, half * 128:(half + 1) * 128], KT[:, half, i * 128:(i + 1) * 128], identb[:]),
                            reads=[Bf("KT"), Bf("identb")], writes=[PTR])
                    P.emit("act", lambda e, i=i: e.activation(out=Ktm[:, i, :], in_=ptr[:, 0:256], func=AF.Copy),
                           reads=[PTR], writes=[Bf(f"Ktm{i}")])
                wt, wb, _ = wload(w_in_d[:, OFF_RV + h * 512: OFF_RV + (h + 1) * 512], D, 512)

                def evV(i, ps, psb):
                    if i == 0:
                        P.emit("act", lambda e: e.activation(out=V[:, 0, :], in_=ps, func=AF.Copy, scale=rowmask),
                               reads=[psb, CST], writes=[Bf("V0")])
                    else:
                        P.emit("act", lambda e: e.activation(out=V[:, i, :], in_=ps, func=AF.Copy),
                               reads=[psb], writes=[Bf(f"V{i}")])
                proj_tm(wt, wb, 512, evV)
                if full:
                    wt, wb, _ = wload(w_in_d[:, OFF_RG + h * 512: OFF_RG + (h + 1) * 512], D, 512)

                    def evG(i, ps, psb):
                        P.emit("act", lambda e: e.activation(out=G[:, i, :], in_=ps, func=AF.Silu),
                               reads=[psb], writes=[Bf(f"G{i}")])
                    proj_tm(wt, wb, 512, evG)
                    P.emit("sp", lambda e, h=h: e.dma_start(out=gngh, in_=gng_d[:, h * 512:(h + 1) * 512]),
                           writes=[Bf("gngh")], dma_buf=Bf("gngh"))
                    if stmode == "slots":
                        for hf in range(2):
                            for s_ in range(3):
                                P.emit("sp", lambda e, s_=s_, hf=hf, h=h: e.dma_start(out=Rs[:, s_, :], in_=Rin_d[s_, h, hf]),
                                       writes=[Bf(f"Rs{s_}")], dma_buf=Bf(f"Rs{s_}"))
                            P.emit("dve", lambda e, hf=hf, h=h: e.scalar_tensor_tensor(
                                out=T32[:, hf, :], in0=Rs[:, 0, :], scalar=rcoef[:, 0, h:h + 1], in1=Rs[:, 1, :],
                                op0=ALU.mult, op1=ALU.add), reads=[Bf("Rs0"), Bf("Rs1"), Bf("rcoef")], writes=[Bf(f"T32_{hf}")])
                            P.emit("dve", lambda e, hf=hf, h=h: e.scalar_tensor_tensor(
                                out=T32[:, hf, :], in0=T32[:, hf, :], scalar=rcoef[:, 1, h:h + 1], in1=Rs[:, 2, :],
                                op0=ALU.mult, op1=ALU.add), reads=[Bf("Rs2"), Bf(f"T32_{hf}"), Bf("rcoef")], writes=[Bf(f"T32_{hf}")])
                            P.emit("dve", lambda e, hf=hf, h=h: e.tensor_scalar(
                                out=T32[:, hf, :], in0=T32[:, hf, :], scalar1=rcoef[:, 2, h:h + 1], scalar2=None, op0=ALU.mult),
                                reads=[Bf(f"T32_{hf}"), Bf("rcoef")], writes=[Bf(f"T32_{hf}")])
                            P.emit("act", lambda e, hf=hf: e.activation(out=Rb[:, hf, :], in_=T32[:, hf, :], func=AF.Copy),
                                   reads=[Bf(f"T32_{hf}")], writes=[Bf(f"Rb{hf}")])
                    elif seg == 0:
                        for hf in range(2):
                            P.emit("dve", lambda e, hf=hf: e.memset(T32[:, hf, :], 0.0), writes=[Bf(f"T32_{hf}")])
                            P.emit("act", lambda e, hf=hf: e.activation(out=Rb[:, hf, :], in_=T32[:, hf, :], func=AF.Copy),
                                   reads=[Bf(f"T32_{hf}")], writes=[Bf(f"Rb{hf}")])
                    else:
                        for hf in range(2):
                            P.emit("sp", lambda e, hf=hf, h=h: e.dma_start(out=T32[:, hf, :], in_=Rst_d[h, hf]),
                                   writes=[Bf(f"T32_{hf}")], dma_buf=Bf(f"T32ld_{hf}"))
                            P.emit("act", lambda e, hf=hf: e.activation(out=Rb[:, hf, :], in_=T32[:, hf, :], func=AF.Copy),
                                   reads=[Bf(f"T32_{hf}")], writes=[Bf(f"Rb{hf}")])
                else:
                    for hf in range(2):
                        P.emit("dve", lambda e, hf=hf: e.memset(T32[:, hf, :], 0.0), writes=[Bf(f"T32_{hf}")])

                for i in range(NTILE):
                    cols = slice(i * 128, (i + 1) * 128)
                    Vi = Bf(f"V{i}")
                    if full:
                        for hf in range(2):
                            P.emit("pe", lambda e, hf=hf, cols=cols: e.matmul(pb[5][:, 0:128], KT[:, hf, cols], QT[:, hf, cols],
                                                                              start=(hf == 0), stop=(hf == 1)),
                                   reads=[Bf("KT"), Bf("QT")], writes=[PB[5]])
                        P.emit("dve", lambda e: e.tensor_tensor(out=AT, in0=pb[5][:, 0:128], in1=causal, op=ALU.mult),
                               reads=[PB[5], CST], writes=[Bf("AT")])
                        P.emit("pe", lambda e, i=i: e.matmul(pb[6][:, :], AT, V[:, i, :], start=True, stop=False),
                               reads=[Bf("AT"), Vi], writes=[PB[6]])
                        for hf in range(2):
                            P.emit("pe", lambda e, hf=hf, cols=cols: e.matmul(pb[6][:, :], QT[:, hf, cols], Rb[:, hf, :],
                                                                              start=False, stop=(hf == 1)),
                                   reads=[Bf("QT"), Bf(f"Rb{hf}")], writes=[PB[6]])
                        P.emit("act", lambda e: e.activation(out=osb2, in_=pb[6][:, :], func=AF.Copy),
                               reads=[PB[6]], writes=[Bf("osb2")])
                        P.emit("dve", lambda e: e.reduce_sum(out=st4[:, 0:1], in_=osb2, axis=AX.X),
                               reads=[Bf("osb2")], writes=[Bf("st_sum")])
                        P.emit("act", lambda e: e.activation(out=junk, in_=osb2, func=AF.Square),
                               reads=[Bf("osb2")], writes=[Bf("junk")])
                        P.emit("dve", lambda e: e.reduce_sum(out=st4[:, 1:2], in_=junk, axis=AX.X),
                               reads=[Bf("junk")], writes=[Bf("st_sq")])
                        P.emit("dve", lambda e: e.tensor_scalar(out=st4[:, 2:3], in0=st4[:, 0:1], scalar1=1.0 / 512, scalar2=None, op0=ALU.mult),
                               reads=[Bf("st_sum")], writes=[Bf("st_mean")])
                        P.emit("dve", lambda e: e.tensor_tensor(out=st4[:, 3:4], in0=st4[:, 2:3], in1=st4[:, 2:3], op=ALU.mult),
                               reads=[Bf("st_mean")], writes=[Bf("st_m2")])
                        P.emit("dve", lambda e: e.scalar_tensor_tensor(out=st4[:, 4:5], in0=st4[:, 1:2], scalar=1.0 / 512, in1=st4[:, 3:4],
                                                                       op0=ALU.mult, op1=ALU.subtract),
                               reads=[Bf("st_sq"), Bf("st_m2")], writes=[Bf("st_var")])
                        rsqrt_ops(st4[:, 5:6], st4[:, 4:5], EPS, [Bf("st_var")], Bf("st_rstd"))
                        P.emit("dve", lambda e: e.tensor_scalar(out=osb, in0=osb2, scalar1=st4[:, 2:3], scalar2=st4[:, 5:6],
                                                                op0=ALU.subtract, op1=ALU.mult),
                               reads=[Bf("osb2"), Bf("st_mean"), Bf("st_rstd")], writes=[Bf("osb")])
                        P.emit("dve", lambda e: e.tensor_tensor(out=osb2, in0=osb, in1=gngh, op=ALU.mult),
                               reads=[Bf("osb"), Bf("gngh")], writes=[Bf("osb2")])
                        P.emit("dve", lambda e, i=i: e.tensor_tensor(out=opb, in0=osb2, in1=G[:, i, :], op=ALU.mult),
                               reads=[Bf("osb2"), Bf(f"G{i}")], writes=[Bf("opb")])
                        for c in range(4):
                            P.emit("pe", lambda e, c=c: e.transpose(ptr[:, (4 + c) * 128:(5 + c) * 128],
                                                                    opb[:, c * 128:(c + 1) * 128], identb[:]),
                                   reads=[Bf("opb"), Bf("identb")], writes=[PTR])
                        for c in range(4):
                            P.emit("act", lambda e, cols=cols, c=c: e.activation(
                                out=oTs[:, c, cols], in_=ptr[:, (4 + c) * 128:(5 + c) * 128], func=AF.Copy),
                                reads=[PTR], writes=[Bf("oTs")])
                    for hf in range(2):
                        P.emit("pe", lambda e, hf=hf, i=i: e.matmul(pb[4][:, :], Ktm[:, i, hf * 128:(hf + 1) * 128], V[:, i, :],
                                                                    start=True, stop=True),
                               reads=[Bf(f"Ktm{i}"), Vi], writes=[PB[4]])
                        if i == 0:
                            P.emit("dve", lambda e, hf=hf: e.tensor_tensor(out=T32[:, hf, :], in0=T32[:, hf, :], in1=pb[4][:, :], op=ALU.add),
                                   reads=[PB[4], Bf(f"T32_{hf}")], writes=[Bf(f"T32_{hf}")])
                        else:
                            dsc = gdec[:, h, 1:2] if i == 1 else gdec[:, h, 0:1]
                            P.emit("dve", lambda e, hf=hf, dsc=dsc: e.scalar_tensor_tensor(
                                out=T32[:, hf, :], in0=T32[:, hf, :], scalar=dsc, in1=pb[4][:, :], op0=ALU.mult, op1=ALU.add),
                                reads=[PB[4], Bf(f"T32_{hf}"), Bf("gdec")], writes=[Bf(f"T32_{hf}")])
                        dnext = gdec[:, h, 1:2] if i == 0 else gdec[:, h, 0:1]
                        if full and i < NTILE - 1:
                            P.emit("act", lambda e, hf=hf, dnext=dnext: e.activation(out=Rb[:, hf, :], in_=T32[:, hf, :], func=AF.Copy, scale=dnext),
                                   reads=[Bf(f"T32_{hf}"), Bf("gdec")], writes=[Bf(f"Rb{hf}")])
                        if stmode == "chain" and seg < 3 and i == NTILE - 1:
                            P.emit("act", lambda e, hf=hf, dnext=dnext: e.activation(out=T32[:, hf, :], in_=T32[:, hf, :], func=AF.Copy, scale=dnext),
                                   reads=[Bf(f"T32_{hf}"), Bf("gdec")], writes=[Bf(f"T32_{hf}")])
                            P.emit("sp", lambda e, hf=hf, h=h: e.dma_start(out=Rst_d[h, hf], in_=T32[:, hf, :]),
                                   reads=[Bf(f"T32_{hf}")], writes=[Bf("Rst_d")], dma_buf=Bf(f"T32st_{hf}"))
                        if (not full) and i == NTILE - 1:
                            P.emit("act", lambda e, hf=hf, dnext=dnext: e.activation(out=T32[:, hf, :], in_=T32[:, hf, :], func=AF.Copy, scale=dnext),
                                   reads=[Bf(f"T32_{hf}"), Bf("gdec")], writes=[Bf(f"T32_{hf}")])
                            final_ops.append(P.emit("sp", lambda e, hf=hf, h=h: e.dma_start(out=Rloc_d[h, hf], in_=T32[:, hf, :]),
                                                    reads=[Bf(f"T32_{hf}")], writes=[Bf("Rloc_d")], dma_buf=Bf(f"T32st_{hf}")))
                if full:
                    for c in range(4):
                        P.emit("sp", lambda e, c=c, h=h: e.dma_start(out=oT_d[h * 4 + c], in_=oTs[:, c, :]),
                               reads=[Bf("oTs")], writes=[Bf(f"oT_d{h * 4 + c}")], dma_buf=Bf(f"oTs_st{c}"))

        def phase_hg():
            sg = ar("sg", [128, NT])
            gl = ar("gl", [128, NT])
            bb = ar("bb", [128, NT])
            eb = ar("eb", [128, NT])
            enb = ar("enb", [128, NT])
            KhT = ar("KhT", [128, NT], BF16)
            V4 = ar("V4", [128, NTILE, 512], BF16)
            Kt4 = ar("Kt4", [128, 4, 128], BF16)
            Th = ar("Th", [128, 128])
            Sb = ar("Sb", [128, 128], BF16)
            if full:
                QhT = ar("QhT", [128, NT], BF16)
                GhT = ar("GhT", [128, NT], BF16)
                sq = ar("sq", [128, NT])
                ATh = ar("ATh", [128, 128], BF16)
                sqb = ar("sqb", [128, 128], BF16)
                rstd = ar("rstd", [128, 128])
                otmp = ar("otmp", [128, 128])
                ohT = ar("ohT", [128, NT], BF16)
                Ss = ar("Ss", [128, 3, 128])
            for gidx in range(_DBG.get('hg_groups', 4)):
                wi_t, wi_b, _ = wload(w_in_d[:, OFF_HI + gidx * 512: OFF_HI + (gidx + 1) * 512], D, 512)

                def evV(i, ps, psb):
                    if i == 0:
                        P.emit("act", lambda e: e.activation(out=V4[:, 0, :], in_=ps, func=AF.Copy, scale=rowmask),
                               reads=[psb, CST], writes=[Bf("V4_0")])
                    else:
                        P.emit("act", lambda e: e.activation(out=V4[:, i, :], in_=ps, func=AF.Copy),
                               reads=[psb], writes=[Bf(f"V4_{i}")])
                proj_tm(wi_t, wi_b, 512, evV)
                for hh in range(4):
                    H = gidx * 4 + hh
                    wf_t, wf_b, _ = wload(w_in_d[:, OFF_HF + H * 128: OFF_HF + (H + 1) * 128], D, 128)

                    def evF(tb, ps, psb):
                        P.emit("act", lambda e: e.activation(out=sg[:, tb * 384:(tb + 1) * 384], in_=ps, func=AF.Sigmoid),
                               reads=[psb], writes=[Bf("sg")])
                    proj_fm(wf_t, wf_b, 0, evF)
                    P.emit("dve", lambda e, H=H: e.tensor_scalar(out=sg, in0=sg, scalar1=omlv[:, H:H + 1], scalar2=lbv[:, H:H + 1],
                                                                 op0=ALU.mult, op1=ALU.add),
                           reads=[Bf("sg")] + LBV, writes=[Bf("sg")])
                    P.emit("act", lambda e: e.activation(out=gl, in_=sg, func=AF.Ln), reads=[Bf("sg")], writes=[Bf("gl")])
                    P.emit("dve", lambda e: e.tensor_tensor(out=gl[:, 0:128], in0=gl[:, 0:128], in1=cmask0, op=ALU.mult),
                           reads=[Bf("gl"), CST], writes=[Bf("gl")])
                    P.emit("dve", lambda e: e.tensor_tensor_scan(out=bb, data0=resetm[:], data1=gl, initial=0.0,
                                                                 op0=ALU.mult, op1=ALU.add),
                           reads=[Bf("gl"), Bf("resetm")], writes=[Bf("bb")])
                    P.emit("act", lambda e: e.activation(out=eb, in_=bb, func=AF.Exp), reads=[Bf("bb")], writes=[Bf("eb")])
                    P.emit("act", lambda e: e.activation(out=enb, in_=bb, func=AF.Exp, scale=-1.0), reads=[Bf("bb")], writes=[Bf("enb")])
                    P.emit("dve", lambda e: e.tensor_scalar(out=sg, in0=sg, scalar1=-1.0, scalar2=1.0, op0=ALU.mult, op1=ALU.add),
                           reads=[Bf("sg")], writes=[Bf("sg")])
                    P.emit("dve", lambda e: e.tensor_tensor(out=KhT, in0=sg, in1=enb, op=ALU.mult),
                           reads=[Bf("sg"), Bf("enb")], writes=[Bf("KhT")])
                    P.emit("dve", lambda e, H=H: e.reduce_sum(out=Lsum[:, H:H + 1], in_=bb[:, 31::32], axis=AX.X),
                           reads=[Bf("bb")], writes=[Bf("Lsum")])
                    if full:
                        wq_t, wq_b, _ = wload(w_in_d[:, OFF_HQ + H * 128: OFF_HQ + (H + 1) * 128], D, 128)

                        def evQ(tb, ps, psb):
                            P.emit("act", lambda e: e.activation(out=sq[:, tb * 384:(tb + 1) * 384], in_=ps, func=AF.Silu),
                                   reads=[psb], writes=[Bf("sq")])
                        proj_fm(wq_t, wq_b, 0, evQ)
                        P.emit("dve", lambda e: e.tensor_tensor(out=QhT, in0=sq, in1=eb, op=ALU.mult),
                               reads=[Bf("sq"), Bf("eb")], writes=[Bf("QhT")])
                        wg_t, wg_b, _ = wload(w_in_d[:, OFF_HG + H * 128: OFF_HG + (H + 1) * 128], D, 128)

                        def evG(tb, ps, psb):
                            P.emit("act", lambda e: e.activation(out=GhT[:, tb * 384:(tb + 1) * 384], in_=ps, func=AF.Silu),
                                   reads=[psb], writes=[Bf("GhT")])
                        proj_fm(wg_t, wg_b, 0, evG)
                        if stmode == "slots":
                            for s_ in range(3):
                                P.emit("sp", lambda e, s_=s_, H=H: e.dma_start(out=Ss[:, s_, :], in_=Sin_d[s_, H]),
                                       writes=[Bf(f"Ss{s_}")], dma_buf=Bf(f"Ss{s_}"))
                            P.emit("dve", lambda e, H=H: e.scalar_tensor_tensor(out=Th, in0=Ss[:, 0, :], scalar=Fsl[:, 1, H:H + 1], in1=Ss[:, 1, :],
                                                                                op0=ALU.mult, op1=ALU.add),
                                   reads=[Bf("Ss0"), Bf("Ss1"), Bf("Fsl")], writes=[Bf("Th")])
                            P.emit("dve", lambda e, H=H: e.scalar_tensor_tensor(out=Th, in0=Th, scalar=Fsl[:, 2, H:H + 1], in1=Ss[:, 2, :],
                                                                                op0=ALU.mult, op1=ALU.add),
                                   reads=[Bf("Th"), Bf("Ss2"), Bf("Fsl")], writes=[Bf("Th")])
                        elif seg == 0:
                            P.emit("dve", lambda e: e.memset(Th, 0.0), writes=[Bf("Th")])
                        else:
                            P.emit("sp", lambda e, H=H: e.dma_start(out=Th, in_=Sst_d[H]), writes=[Bf("Th")], dma_buf=Bf("Thld"))
                    else:
                        P.emit("dve", lambda e: e.memset(Th, 0.0), writes=[Bf("Th")])
                    Eprev = None
                    for i in range(NTILE):
                        cols = slice(i * 128, (i + 1) * 128)
                        Vi = Bf(f"V4_{i}")
                        vsl = V4[:, i, hh * 128:(hh + 1) * 128]
                        P.emit("pe", lambda e, cols=cols: e.transpose(ptr[:, 512:640], KhT[:, cols], identb[:]),
                               reads=[Bf("KhT"), Bf("identb")], writes=[PTR])
                        for j in range(4):
                            P.emit("act", lambda e, j=j: e.activation(out=Kt4[:, j, :], in_=ptr[:, 512:640], func=AF.Copy, scale=ind[:, j:j + 1]),
                                   reads=[PTR, CST], writes=[Bf(f"Kt4_{j}")])
                        for j in range(4):
                            P.emit("pe", lambda e, j=j, vsl=vsl: e.matmul(pb[4][:, j * 128:(j + 1) * 128], Kt4[:, j, :], vsl, start=True, stop=True),
                                   reads=[Bf(f"Kt4_{j}"), Vi], writes=[PB[4]])
                        if full:
                            P.emit("pe", lambda e, cols=cols: e.matmul(pb[5][:, 0:128], KhT[:, cols], QhT[:, cols], start=True, stop=True),
                                   reads=[Bf("KhT"), Bf("QhT")], writes=[PB[5]])
                            P.emit("dve", lambda e: e.tensor_tensor(out=ATh, in0=pb[5][:, 0:128], in1=bdmask, op=ALU.mult),
                                   reads=[PB[5], CST], writes=[Bf("ATh")])
                            P.emit("pe", lambda e, vsl=vsl: e.matmul(pb[6][:, 0:128], vsl, ATh, start=True, stop=False),
                                   reads=[Vi, Bf("ATh")], writes=[PB[6]])
                        for j in range(4):
                            c0 = i * 128 + j * 32
                            if full:
                                if Eprev is None:
                                    P.emit("act", lambda e: e.activation(out=Sb, in_=Th, func=AF.Copy), reads=[Bf("Th")], writes=[Bf("Sb")])
                                else:
                                    P.emit("act", lambda e, Eprev=Eprev: e.activation(out=Sb, in_=Th, func=AF.Copy, scale=Eprev),
                                           reads=[Bf("Th"), Bf("eb")], writes=[Bf("Sb")])
                                P.emit("pe", lambda e, j=j, c0=c0: e.matmul(pb[6][:, j * 32:(j + 1) * 32], Sb, QhT[:, c0:c0 + 32],
                                                                            start=False, stop=(j == 3)),
                                       reads=[Bf("Sb"), Bf("QhT")], writes=[PB[6]])
                            if Eprev is None:
                                P.emit("dve", lambda e, j=j: e.tensor_tensor(out=Th, in0=Th, in1=pb[4][:, j * 128:(j + 1) * 128], op=ALU.add),
                                       reads=[PB[4], Bf("Th")], writes=[Bf("Th")])
                            else:
                                P.emit("dve", lambda e, j=j, Eprev=Eprev: e.scalar_tensor_tensor(
                                    out=Th, in0=Th, scalar=Eprev, in1=pb[4][:, j * 128:(j + 1) * 128], op0=ALU.mult, op1=ALU.add),
                                    reads=[PB[4], Bf("Th"), Bf("eb")], writes=[Bf("Th")])
                            Eprev = eb[:, c0 + 31:c0 + 32]
                        if full:
                            P.emit("act", lambda e: e.activation(out=sqb, in_=pb[6][:, 0:128], func=AF.Square), reads=[PB[6]], writes=[Bf("sqb")])
                            P.emit("pe", lambda e: e.matmul(pb[5][:, 128:256], onesb[:], sqb, start=True, stop=True),
                                   reads=[Bf("sqb"), Bf("onesb")], writes=[PB[5]])
                            rsqrt_ops(rstd, pb[5][:, 128:256], 128.0 * EPS, [PB[5]], Bf("rstd"))
                            P.emit("dve", lambda e: e.tensor_tensor(out=otmp, in0=pb[6][:, 0:128], in1=rstd, op=ALU.mult),
                                   reads=[PB[6], Bf("rstd")], writes=[Bf("otmp")])
                            P.emit("dve", lambda e, H=H, cols=cols: e.scalar_tensor_tensor(
                                out=ohT[:, cols], in0=otmp, scalar=hgs[:, H:H + 1], in1=GhT[:, cols], op0=ALU.mult, op1=ALU.mult),
                                reads=[Bf("otmp"), Bf("hgs"), Bf("GhT")], writes=[Bf("ohT")])
                    if full:
                        P.emit("sp", lambda e, H=H: e.dma_start(out=oT_d[32 + H], in_=ohT),
                               reads=[Bf("ohT")], writes=[Bf(f"oT_d{32 + H}")], dma_buf=Bf("ohT_st"))
                        if stmode == "chain" and seg < 3:
                            P.emit("act", lambda e, Eprev=Eprev: e.activation(out=Th, in_=Th, func=AF.Copy, scale=Eprev),
                                   reads=[Bf("Th"), Bf("eb")], writes=[Bf("Th")])
                            P.emit("sp", lambda e, H=H: e.dma_start(out=Sst_d[H], in_=Th),
                                   reads=[Bf("Th")], writes=[Bf("Sst_d")], dma_buf=Bf("Th_st"))
                    else:
                        P.emit("act", lambda e, Eprev=Eprev: e.activation(out=Th, in_=Th, func=AF.Copy, scale=Eprev),
                               reads=[Bf("Th"), Bf("eb")], writes=[Bf("Th")])
                        final_ops.append(P.emit("sp", lambda e, H=H: e.dma_start(out=Sloc_d[H], in_=Th),
                                                reads=[Bf("Th")], writes=[Bf("Sloc_d")], dma_buf=Bf("Th_st")))

        if "mix" in phases:
            phase_ret()
            arena_reset()
            phase_hg()
            if not full:
                final_ops.append(P.emit("sp", lambda e: e.dma_start(out=Lsum_d, in_=Lsum[:]),
                                        reads=[Bf("Lsum")], writes=[Bf("Lsum_d")], dma_buf=Bf("Lsum_st")))

        def acc_mm(bankpair, units, ncol0, rhs_fn, rhs_bufs):
            total = sum(u[2] for u in units)
            for hb in range(2):
                k = 0
                for (wt, wb, kcn) in units:
                    for kc in range(kcn):
                        P.emit("pe", lambda e, wt=wt, kc=kc, k=k, hb=hb: e.matmul(
                            pb[bankpair + hb][:, 0:HB], wt[:, kc, ncol0:ncol0 + 128], rhs_fn(k, hb),
                            start=(k == 0), stop=(k == total - 1)),
                            reads=[wb] + rhs_bufs, writes=[PB[bankpair + hb]])
                        k += 1

        def phase_tok(tb):
            t0 = tb * TB
            tcols = slice(t0, t0 + TB)
            mg_ = ar("merged", [128, 16, TB], BF16)
            z = ar("z", [128, 16, TB])
            mark = ast["off"]
            orT = ar("orT", [128, 32, TB], BF16)
            ohT2 = ar("ohT2", [128, 16, TB], BF16)
            sga = ar("sga", [128, TB])
            t1 = ar("t1", [128, TB])
            for c4 in range(8):
                P.emit("sp", lambda e, c4=c4: e.dma_start(out=orT[:, c4 * 4:(c4 + 1) * 4, :],
                                                          in_=oT_d[c4 * 4:(c4 + 1) * 4, :, tcols].rearrange("c p t -> p c t")),
                       reads=[Bf(f"oT_d{c4 * 4 + i}") for i in range(4)], writes=[Bf(f"orT{c4}")], dma_buf=Bf(f"orT{c4}"))
            for c4 in range(4):
                P.emit("sp", lambda e, c4=c4: e.dma_start(out=ohT2[:, c4 * 4:(c4 + 1) * 4, :],
                                                          in_=oT_d[32 + c4 * 4:32 + (c4 + 1) * 4, :, tcols].rearrange("c p t -> p c t")),
                       reads=[Bf(f"oT_d{32 + c4 * 4 + i}") for i in range(4)], writes=[Bf(f"ohT2{c4}")], dma_buf=Bf(f"ohT2{c4}"))
            ORT = [Bf(f"orT{c}") for c in range(8)]
            OHT = [Bf(f"ohT2{c}") for c in range(4)]
            for ng in range(4):
                ncs = slice(ng * 512, (ng + 1) * 512)
                u_ro = [wload(w_ro_d[0:2048, ncs], 2048, 512), wload(w_ro_d[2048:4096, ncs], 2048, 512)]
                u_ga = [wload(w_in_d[:, OFF_GA + ng * 512: OFF_GA + (ng + 1) * 512], D, 512)]
                for nn in range(4):
                    n = ng * 4 + nn
                    acc_mm(0, u_ro, nn * 128, lambda k, hb: orT[:, k, hb * HB:(hb + 1) * HB], ORT)
                    acc_mm(2, u_ga, nn * 128, lambda k, hb: hTb[:, k, t0 + hb * HB: t0 + (hb + 1) * HB], HTB)
                    for hb in range(2):
                        P.emit("act", lambda e, hb=hb: e.activation(out=sga[:, hb * HB:(hb + 1) * HB], in_=pb[2 + hb][:, 0:HB], func=AF.Sigmoid),
                               reads=[PB[2 + hb]], writes=[Bf("sga")])
                        P.emit("dve", lambda e, hb=hb, n=n: e.tensor_tensor(out=z[:, n, hb * HB:(hb + 1) * HB], in0=pb[hb][:, 0:HB],
                                                                            in1=sga[:, hb * HB:(hb + 1) * HB], op=ALU.mult),
                               reads=[PB[hb], Bf("sga")], writes=[Bf(f"z{n}")])
            for ng in range(4):
                ncs = slice(ng * 512, (ng + 1) * 512)
                u_ho = [wload(w_ho_d[:, ncs], D, 512)]
                u_gb = [wload(w_in_d[:, OFF_GB + ng * 512: OFF_GB + (ng + 1) * 512], D, 512)]
                for nn in range(4):
                    n = ng * 4 + nn
                    acc_mm(0, u_ho, nn * 128, lambda k, hb: ohT2[:, k, hb * HB:(hb + 1) * HB], OHT)
                    acc_mm(2, u_gb, nn * 128, lambda k, hb: hTb[:, k, t0 + hb * HB: t0 + (hb + 1) * HB], HTB)
                    for hb in range(2):
                        P.emit("act", lambda e, hb=hb: e.activation(out=sga[:, hb * HB:(hb + 1) * HB], in_=pb[2 + hb][:, 0:HB], func=AF.Sigmoid),
                               reads=[PB[2 + hb]], writes=[Bf("sga")])
                        P.emit("dve", lambda e, hb=hb: e.tensor_tensor(out=t1[:, hb * HB:(hb + 1) * HB], in0=pb[hb][:, 0:HB],
                                                                       in1=sga[:, hb * HB:(hb + 1) * HB], op=ALU.mult),
                               reads=[PB[hb], Bf("sga")], writes=[Bf("t1")])
                    P.emit("dve", lambda e, n=n: e.tensor_tensor(out=mg_[:, n, :], in0=z[:, n, :], in1=t1, op=ALU.add),
                           reads=[Bf("t1"), Bf(f"z{n}")], writes=[Bf(f"mg{n}")])
            P.barrier()
            ast["off"] = mark
            zsq = ar("zsq", [128, TB])
            mean = ar("mean", [128, TB])
            rs = ar("rs", [128, TB])
            nmr = ar("nmr", [128, TB])
            MG = [Bf(f"mg{n}") for n in range(16)]
            for n4 in range(4):
                P.emit("sp", lambda e, n4=n4: e.dma_start(out=z[:, n4 * 4:(n4 + 1) * 4, :],
                                                          in_=hT_d[n4 * 512:(n4 + 1) * 512, tcols].rearrange("(c p) t -> p c t", p=128)),
                       writes=[Bf(f"z{n4 * 4 + i}") for i in range(4)], dma_buf=Bf(f"zld{n4}"))
            for ng in range(4):
                u_o = [wload(w_o_d[:, ng * 512:(ng + 1) * 512], D, 512)]
                for nn in range(4):
                    n = ng * 4 + nn
                    acc_mm(0, u_o, nn * 128, lambda k, hb: mg_[:, k, hb * HB:(hb + 1) * HB], MG)
                    for hb in range(2):
                        P.emit("dve", lambda e, n=n, hb=hb: e.scalar_tensor_tensor(
                            out=z[:, n, hb * HB:(hb + 1) * HB], in0=z[:, n, hb * HB:(hb + 1) * HB], scalar=ALPHA, in1=pb[hb][:, 0:HB],
                            op0=ALU.mult, op1=ALU.add), reads=[PB[hb], Bf(f"z{n}")], writes=[Bf(f"z{n}")])

            def after1(n):
                P.emit("act", lambda e, n=n: e.activation(out=hTb[:, n, tcols], in_=z[:, n, :], func=AF.Copy),
                       reads=[Bf(f"z{n}")], writes=[HTB[n // 4]])
                P.emit("sp", lambda e, n=n: e.dma_start(out=h1_d[n * 128:(n + 1) * 128, tcols], in_=z[:, n, :]),
                       reads=[Bf(f"z{n}")], writes=[Bf(f"h1d{n}")], dma_buf=Bf(f"zst{n}"))
            layer_norm(lambda n, cs: z[:, n, cs], lambda n: Bf(f"z{n}"), zsq, mean, rs, nmr, 0, 1, after1)

        def layer_norm(zf, zb, zsq, mean, rs, nmr, gi, bi, after):
            for hb in range(2):
                cs = slice(hb * HB, (hb + 1) * HB)
                for n in range(16):
                    P.emit("pe", lambda e, n=n, cs=cs: e.matmul(pb[4][:, 0:HB], onesf, zf(n, cs), start=(n == 0), stop=(n == 15)),
                           reads=[zb(n), CST], writes=[PB[4]])
                for n in range(16):
                    P.emit("act", lambda e, n=n, cs=cs: e.activation(out=zsq[:, 0:HB], in_=zf(n, cs), func=AF.Square),
                           reads=[zb(n)], writes=[Bf("zsq")])
                    P.emit("pe", lambda e, n=n: e.matmul(pb[5][:, 0:HB], onesf, zsq[:, 0:HB], start=(n == 0), stop=(n == 15)),
                           reads=[Bf("zsq"), CST], writes=[PB[5]])
                P.emit("dve", lambda e, cs=cs: e.tensor_scalar(out=mean[:, cs], in0=pb[4][:, 0:HB], scalar1=1.0 / D, scalar2=None, op0=ALU.mult),
                       reads=[PB[4]], writes=[Bf("mean")])
                P.emit("dve", lambda e, cs=cs: e.tensor_tensor(out=nmr[:, cs], in0=mean[:, cs], in1=mean[:, cs], op=ALU.mult),
                       reads=[Bf("mean")], writes=[Bf("nmr")])
                P.emit("dve", lambda e, cs=cs: e.scalar_tensor_tensor(out=rs[:, cs], in0=pb[5][:, 0:HB], scalar=1.0 / D, in1=nmr[:, cs],
                                                                      op0=ALU.mult, op1=ALU.subtract),
                       reads=[PB[5], Bf("nmr")], writes=[Bf("rs")])
                rsqrt_ops(rs[:, cs], rs[:, cs], EPS, [Bf("rs")], Bf("rs"))
            allc = slice(0, TB)
            for n in range(16):
                P.emit("dve", lambda e, n=n: e.tensor_tensor(out=zf(n, allc), in0=zf(n, allc), in1=mean, op=ALU.subtract),
                       reads=[zb(n), Bf("mean")], writes=[zb(n)])
                P.emit("dve", lambda e, n=n: e.tensor_tensor(out=zf(n, allc), in0=zf(n, allc), in1=rs, op=ALU.mult),
                       reads=[zb(n), Bf("rs")], writes=[zb(n)])
                P.emit("dve", lambda e, n=n: e.tensor_scalar(out=zf(n, allc), in0=zf(n, allc), scalar1=lnp[:, gi, n:n + 1], scalar2=lnp[:, bi, n:n + 1],
                                                             op0=ALU.mult, op1=ALU.add),
                       reads=[zb(n), Bf("lnp")], writes=[zb(n)])
                after(n)

        def phase_ffn():
            zA = ar("zA", [128, 16, NT])
            act = ar("actT", [128, 4, NT], BF16)
            t1 = ar("t1b", [128, TB])
            t2 = ar("t2b", [128, TB])
            zsq = ar("zsq", [128, TB])
            mean = ar("mean", [128, TB])
            rs = ar("rs", [128, TB])
            nmr = ar("nmr", [128, TB])
            ZA = [Bf(f"zA{n}") for n in range(16)]
            for n4 in range(4):
                P.emit("sp", lambda e, n4=n4: e.dma_start(out=zA[:, n4 * 4:(n4 + 1) * 4, :],
                                                          in_=h1_d[n4 * 512:(n4 + 1) * 512, :].rearrange("(c p) t -> p c t", p=128)),
                       reads=[Bf(f"h1d{n4 * 4 + i}") for i in range(4)], writes=[ZA[n4 * 4 + i] for i in range(4)], dma_buf=Bf(f"zAld{n4}"))
            for n in range(16):
                P.emit("act", lambda e, n=n: e.activation(out=zA[:, n, :], in_=zA[:, n, :], func=AF.Copy, scale=ALPHA),
                       reads=[ZA[n]], writes=[ZA[n]])

            def ffn(wg_ap, wu_ap, wd_ap, F, gate=None):
                nchunks = F // 128
                c = 0
                while c < nchunks:
                    gsz = min(4, nchunks - c)
                    fc0 = c * 128
                    u_g = [wload(wg_ap[:, fc0:fc0 + gsz * 128], D, gsz * 128)]
                    u_u = [wload(wu_ap[:, fc0:fc0 + gsz * 128], D, gsz * 128)]
                    for j in range(gsz):
                        for pr in range(NTB):
                            p0 = pr * TB
                            acc_mm(0, u_g, j * 128, lambda k, hb, p0=p0: hTb[:, k, p0 + hb * HB:p0 + (hb + 1) * HB], HTB)
                            for hb in range(2):
                                P.emit("act", lambda e, hb=hb: e.activation(out=t1[:, hb * HB:(hb + 1) * HB], in_=pb[hb][:, 0:HB], func=AF.Silu),
                                       reads=[PB[hb]], writes=[Bf("t1")])
                            acc_mm(2, u_u, j * 128, lambda k, hb, p0=p0: hTb[:, k, p0 + hb * HB:p0 + (hb + 1) * HB], HTB)
                            for hb in range(2):
                                dsl = slice(p0 + hb * HB, p0 + (hb + 1) * HB)
                                if gate is None:
                                    P.emit("dve", lambda e, hb=hb, j=j, dsl=dsl: e.tensor_tensor(
                                        out=act[:, j, dsl], in0=t1[:, hb * HB:(hb + 1) * HB], in1=pb[2 + hb][:, 0:HB], op=ALU.mult),
                                        reads=[PB[2 + hb], Bf("t1")], writes=[Bf(f"act{j}")])
                                else:
                                    P.emit("dve", lambda e, hb=hb: e.tensor_tensor(
                                        out=t2[:, hb * HB:(hb + 1) * HB], in0=t1[:, hb * HB:(hb + 1) * HB], in1=pb[2 + hb][:, 0:HB], op=ALU.mult),
                                        reads=[PB[2 + hb], Bf("t1")], writes=[Bf("t2")])
                                    P.emit("dve", lambda e, hb=hb, j=j, dsl=dsl: e.tensor_tensor(
                                        out=act[:, j, dsl], in0=t2[:, hb * HB:(hb + 1) * HB], in1=gate[:, dsl], op=ALU.mult),
                                        reads=[Bf("t2"), Bf("gbe")], writes=[Bf(f"act{j}")])
                    ACT = [Bf(f"act{a}") for a in range(gsz)]
                    for ng in range(4):
                        u_d = [wload(wd_ap[c * 128:(c + gsz) * 128, ng * 512:(ng + 1) * 512], gsz * 128, 512)]
                        for nn in range(4):
                            n = ng * 4 + nn
                            for pr in range(NTB):
                                p0 = pr * TB
                                acc_mm(0, u_d, nn * 128, lambda k, hb, p0=p0: act[:, k, p0 + hb * HB:p0 + (hb + 1) * HB], ACT)
                                for hb in range(2):
                                    dsl = slice(p0 + hb * HB, p0 + (hb + 1) * HB)
                                    P.emit("dve", lambda e, n=n, hb=hb, dsl=dsl: e.tensor_tensor(
                                        out=zA[:, n, dsl], in0=zA[:, n, dsl], in1=pb[hb][:, 0:HB], op=ALU.add),
                                        reads=[PB[hb], ZA[n]], writes=[ZA[n]])
                    c += gsz

            if not moe:
                ffn(wg_d, wu_d, wd_d, DFF)
            else:
                NG = NT // 96
                wrf = ar("wrf", [128, 16, NE])
                P.emit("sp", lambda e: e.dma_start(out=wrf, in_=wr_d.rearrange("(c p) n -> p c n", p=128)),
                       writes=[Bf("wrf")], dma_buf=Bf("wrf"))
                lg = ar("lg", [128, 16])
                gts = ar("gts", [128, 4, NE])
                gtA = ar("gtA", [128, NG, NE])
                gexp = ar("gexp", [128, 128])
                gbe = ar("gbe", [128, NT], BF16)
                identf = cst[:, 2, :]
                for gq in range(NG):
                    ts_ = slice(gq * 96, (gq + 1) * 96)
                    for kc in range(16):
                        P.emit("pe", lambda e, kc=kc, ts_=ts_: e.matmul(pb[4][0:96, 0:NE], zA[:, kc, ts_], wrf[:, kc, :],
                                                                        start=(kc == 0), stop=(kc == 15)),
                               reads=[ZA[kc], Bf("wrf")], writes=[PB[4]])
                    L = gts[0:96, 0, :]
                    P.emit("act", lambda e, L=L: e.activation(out=L, in_=pb[4][0:96, 0:NE], func=AF.Copy, scale=1.0 / ALPHA),
                           reads=[PB[4]], writes=[Bf("gts0")])
                    m1 = lg[0:96, 0:1]
                    m2 = lg[0:96, 1:2]
                    P.emit("dve", lambda e, L=L, m1=m1: e.reduce_max(out=m1, in_=L, axis=AX.X), reads=[Bf("gts0")], writes=[Bf("lg_m1")])
                    E1 = gts[0:96, 1, :]
                    P.emit("dve", lambda e, L=L, m1=m1, E1=E1: e.tensor_scalar(out=E1, in0=L, scalar1=m1, scalar2=None, op0=ALU.is_equal),
                           reads=[Bf("gts0"), Bf("lg_m1")], writes=[Bf("gts1")])
                    L2 = gts[0:96, 2, :]
                    P.emit("dve", lambda e, L=L, E1=E1, L2=L2: e.scalar_tensor_tensor(out=L2, in0=E1, scalar=-1e30, in1=L, op0=ALU.mult, op1=ALU.add),
                           reads=[Bf("gts0"), Bf("gts1")], writes=[Bf("gts2")])
                    P.emit("dve", lambda e, L2=L2, m2=m2: e.reduce_max(out=m2, in_=L2, axis=AX.X), reads=[Bf("gts2")], writes=[Bf("lg_m2")])
                    E2 = gts[0:96, 3, :]
                    P.emit("dve", lambda e, L2=L2, m2=m2, E2=E2: e.tensor_scalar(out=E2, in0=L2, scalar1=m2, scalar2=None, op0=ALU.is_equal),
                           reads=[Bf("gts2"), Bf("lg_m2")], writes=[Bf("gts3")])
                    dd_ = lg[0:96, 2:3]
                    w1 = lg[0:96, 3:4]
                    w2 = lg[0:96, 4:5]
                    P.emit("dve", lambda e, m1=m1, m2=m2, dd_=dd_: e.tensor_tensor(out=dd_, in0=m2, in1=m1, op=ALU.subtract),
                           reads=[Bf("lg_m1"), Bf("lg_m2")], writes=[Bf("lg_dd")])
                    P.emit("act", lambda e, dd_=dd_: e.activation(out=dd_, in_=dd_, func=AF.Exp), reads=[Bf("lg_dd")], writes=[Bf("lg_dd")])
                    P.emit("dve", lambda e, dd_=dd_: e.tensor_scalar(out=dd_, in0=dd_, scalar1=1.0, scalar2=None, op0=ALU.add),
                           reads=[Bf("lg_dd")], writes=[Bf("lg_dd")])
                    P.emit("dve", lambda e, dd_=dd_, w1=w1: e.reciprocal(out=w1, in_=dd_), reads=[Bf("lg_dd")], writes=[Bf("lg_w1")])
                    P.emit("dve", lambda e, w1=w1, w2=w2: e.tensor_scalar(out=w2, in0=w1, scalar1=-1.0, scalar2=1.0, op0=ALU.mult, op1=ALU.add),
                           reads=[Bf("lg_w1")], writes=[Bf("lg_w2")])
                    Gt = gtA[0:96, gq, :]
                    P.emit("dve", lambda e, E1=E1, w1=w1, Gt=Gt: e.tensor_scalar(out=Gt, in0=E1, scalar1=w1, scalar2=None, op0=ALU.mult),
                           reads=[Bf("gts1"), Bf("lg_w1")], writes=[Bf("gtA")])
                    P.emit("dve", lambda e, E2=E2, w2=w2, Gt=Gt: e.scalar_tensor_tensor(out=Gt, in0=E2, scalar=w2, in1=Gt, op0=ALU.mult, op1=ALU.add),
                           reads=[Bf("gts3"), Bf("lg_w2"), Bf("gtA")], writes=[Bf("gtA")])
                for ee in range(NE):
                    for gq in range(NG):
                        ts_ = slice(gq * 96, (gq + 1) * 96)
                        P.emit("dve", lambda e, ee=ee, gq=gq: e.tensor_scalar(out=gexp[0:96, :], in0=onesf[0:96, :], scalar1=gtA[0:96, gq, ee:ee + 1],
                                                                             scalar2=None, op0=ALU.mult),
                               reads=[Bf("gtA"), CST], writes=[Bf("gexp")])
                        P.emit("pe", lambda e: e.matmul(pb[5][:, 0:96], gexp[0:96, :], identf[0:96, 0:96], start=True, stop=True),
                               reads=[Bf("gexp"), CST], writes=[PB[5]])
                        P.emit("act", lambda e, ts_=ts_: e.activation(out=gbe[:, ts_], in_=pb[5][:, 0:96], func=AF.Copy),
                               reads=[PB[5]], writes=[Bf("gbe")])
                    ffn(mg_d[ee], mu_d[ee], md_d[ee], DFE, gate=gbe)

            for pr in range(NTB):
                p0 = pr * TB

                def store(n, p0=p0):
                    final_ops.append(P.emit("sp", lambda e, n=n: e.dma_start(out=hout_d[n * 128:(n + 1) * 128, p0:p0 + TB], in_=zA[:, n, p0:p0 + TB]),
                                            reads=[ZA[n]], writes=[Bf("hout")], dma_buf=Bf(f"zst{n}")))
                layer_norm(lambda n, cs, p0=p0: zA[:, n, p0 + cs.start:p0 + cs.stop], lambda n: ZA[n], zsq, mean, rs, nmr, 2, 3, store)

        if full and "tok" in phases:
            for tb in range(NTB):
                arena_reset()
                phase_tok(tb)
            arena_reset()
            phase_ffn()

    if kind in ("A", "B"):
        full_ = kind == "B"
        dd = dict(hT=din("hT", [D, NT]), w_in=din("w_in", [D, P_IN]), lb_logits=din("lb_logits", [128, 4, 16]),
                  lbsel=din("lbsel", [128, 4]), rope=din("rope", [8, 4, 128, NT]), cst=din("cst", [128, 6, 128]),
                  resetm=din("resetm", [128, NT]), gdec=din("gdec", [128, 8, 2]))
        if full_:
            dd.update(Rin=din("Rin", [3, 8, 2, 128, 512]), Sin=din("Sin", [3, 16, 128, 128]), Lsl=din("Lsl", [128, 3, 16]),
                      rcoef=din("rcoef", [128, 3, 8]), w_ret_out=din("w_ret_out", [4096, D]), w_hg_out=din("w_hg_out", [D, D]),
                      w_o=din("w_o", [D, D]), gn_g_b=din("gn_g_b", [128, 4096]), hgn_g=din("hgn_g", [128, 16]), ln=din("ln", [128, 4, 16]))
            if moe:
                dd.update(wr=din("wr", [D, NE]), mg=din("mg", [NE, D, DFE]), mu=din("mu", [NE, D, DFE]), md=din("md", [NE, DFE, D]))
            else:
                dd.update(wg=din("wg", [D, DFF]), wu=din("wu", [D, DFF]), wd=din("wd", [DFF, D]))
            dd.update(hT_out=dout("hT_out", [D, NT]), oT_scr=dscr("oT_scr", [48, 128, NT], BF16), h1_scr=dscr("h1_scr", [D, NT]))
        else:
            dd.update(Rloc=dout("Rloc", [8, 2, 128, 512]), Sloc=dout("Sloc", [16, 128, 128]), Lsum=dout("Lsum", [128, 16]))
        body(full_, moe, dd, "slots" if full_ else "zero", 0)
        P.finalize(final_wait_ops=final_ops)
        return nc

    if kind == "BA":
        ddB = dict(hT=din("hT", [D, NT]), w_in=din("w_in", [D, P_IN]), lb_logits=din("lb_logits", [128, 4, 16]),
                   lbsel=din("lbsel", [128, 4]), rope=din("rope", [8, 4, 128, NT]), cst=din("cst", [128, 6, 128]),
                   resetm=din("resetm", [128, NT]), gdec=din("gdec", [128, 8, 2]),
                   Rin=din("Rin", [3, 8, 2, 128, 512]), Sin=din("Sin", [3, 16, 128, 128]), Lsl=din("Lsl", [128, 3, 16]),
                   rcoef=din("rcoef", [128, 3, 8]), w_ret_out=din("w_ret_out", [4096, D]), w_hg_out=din("w_hg_out", [D, D]),
                   w_o=din("w_o", [D, D]), gn_g_b=din("gn_g_b", [128, 4096]), hgn_g=din("hgn_g", [128, 16]), ln=din("ln", [128, 4, 16]))
        if moe:
            ddB.update(wr=din("wr", [D, NE]), mg=din("mg", [NE, D, DFE]), mu=din("mu", [NE, D, DFE]), md=din("md", [NE, DFE, D]))
        else:
            ddB.update(wg=din("wg", [D, DFF]), wu=din("wu", [D, DFF]), wd=din("wd", [DFF, D]))
        ddB.update(hT_out=dout("hT_out", [D, NT]), oT_scr=dscr("oT_scr", [48, 128, NT], BF16), h1_scr=dscr("h1_scr", [D, NT]))
        ddA = dict(hT=ddB["hT_out"], w_in=din("w_in_next", [D, P_IN]), lb_logits=ddB["lb_logits"], lbsel=din("lbsel_next", [128, 4]),
                   rope=ddB["rope"], cst=ddB["cst"], resetm=ddB["resetm"], gdec=ddB["gdec"],
                   Rloc=dout("Rloc", [8, 2, 128, 512]), Sloc=dout("Sloc", [16, 128, 128]), Lsum=dout("Lsum", [128, 16]))
        body(True, moe, ddB, "slots", 0)
        P.barrier()
        body(False, False, ddA, "zero", 0)
        P.finalize(final_wait_ops=final_ops)
        return nc

    NL = _DBG.get("nl", 4)
    NS = _DBG.get("ns", 4)
    hT_all = din("hT", [4, D, NT])
    w_in_all = din("w_in", [4, D, P_IN])
    lbl_d = din("lb_logits", [128, 4, 16])
    lbsel_all = din("lbsel", [4, 128, 4])
    rope_all = din("rope", [4, 8, 4, 128, NT])
    cst_all = din("cst", [4, 128, 6, 128])
    resetm_d = din("resetm", [128, NT])
    gdec_all = din("gdec", [4, 128, 8, 2])
    w_ro_all = din("w_ret_out", [4, 4096, D])
    w_ho_all = din("w_hg_out", [4, D, D])
    w_o_all = din("w_o", [4, D, D])
    gng_all = din("gn_g_b", [4, 128, 4096])
    hgn_all = din("hgn_g", [4, 128, 16])
    ln_all = din("ln", [4, 128, 4, 16])
    wr_all = din("wr", [2, D, NE])
    mg_all = din("mg", [2, NE, D, DFE])
    mu_all = din("mu", [2, NE, D, DFE])
    md_all = din("md", [2, NE, DFE, D])
    wg_all = din("wg", [2, D, DFF])
    wu_all = din("wu", [2, D, DFF])
    wd_all = din("wd", [2, DFF, D])
    hout_all = dout("hT_out", [4, D, NT])
    hbuf = dscr("hbuf", [2, 4, D, NT])
    oT_scr = dscr("oT_scr", [48, 128, NT], BF16)
    h1_scr = dscr("h1_scr", [D, NT])
    Rst = dscr("Rst", [4, 8, 2, 128, 512])
    Sst = dscr("Sst", [4, 16, 128, 128])
    for seg in range(NS):
        for l in range(NL):
            moe_l = (l % 2 == 1)
            dd = dict(hT=(hT_all[seg] if l == 0 else hbuf[(l - 1) % 2, seg]), w_in=w_in_all[l], lb_logits=lbl_d,
                      lbsel=lbsel_all[l], rope=rope_all[seg], cst=cst_all[seg], resetm=resetm_d, gdec=gdec_all[seg],
                      w_ret_out=w_ro_all[l], w_hg_out=w_ho_all[l], w_o=w_o_all[l], gn_g_b=gng_all[l], hgn_g=hgn_all[l],
                      ln=ln_all[l], hT_out=(hout_all[seg] if l == NL - 1 else hbuf[l % 2, seg]), oT_scr=oT_scr, h1_scr=h1_scr,
                      Rst=Rst[l], Sst=Sst[l])
            if moe_l:
                dd.update(wr=wr_all[l // 2], mg=mg_all[l // 2], mu=mu_all[l // 2], md=md_all[l // 2])
            else:
                dd.update(wg=wg_all[l // 2], wu=wu_all[l // 2], wd=wd_all[l // 2])
            P.barrier()
            body(True, moe_l, dd, "chain", seg)
    P.barrier()
    P.finalize(final_wait_ops=[])
    return nc


_PROGS = {}


def _prog(kind, moe=False):
    key = (kind, moe)
    if key not in _PROGS:
        _PROGS[key] = build(kind, moe)
    return _PROGS[key]


def _consts(rank):
    s = np.arange(128)
    causal = (s[:, None] <= s[None, :]).astype(np.float32)
    bd = causal * ((s[:, None] // 32) == (s[None, :] // 32))
    ident = np.eye(128, dtype=np.float32)
    cm0 = np.zeros((128, 128), np.float32)
    if rank == 0:
        cm0[:, 112:] = 1.0
    ones = np.ones((128, 128), np.float32)
    pk = np.zeros((128, 128), np.float32)
    for j in range(4):
        pk[j * 32:(j + 1) * 32, j] = 1.0
    if rank == 0:
        pk[112:, 4] = 1.0
    cst = np.ascontiguousarray(np.stack([causal, bd.astype(np.float32), ident, cm0, ones, pk], axis=1))
    resetm = np.ones((128, NT), np.float32)
    resetm[:, 0::32] = 0.0
    lg = np.log1p(-np.exp2(-5.0 - np.arange(8, dtype=np.float64)))
    gdec = np.zeros((128, 8, 2), np.float32)
    gdec[:, :, 0] = np.exp(128 * lg)[None, :]
    gdec[:, :, 1] = np.exp(128 * lg)[None, :] if rank == 0 else 1.0
    pos = np.zeros(NT, np.float64)
    if rank == 0:
        pos[112:128] = np.arange(16)
    pos[128:] = 16 + 1024 * rank + np.arange(1024)
    inv = 1.0 / (10000.0 ** np.linspace(0.0, 1.0, 128))
    ang = inv[:, None] * pos[None, :]
    cos, sin = np.cos(ang), np.sin(ang)
    j = (np.arange(NT) % 128).astype(np.float64)
    rope = np.zeros((8, 4, 128, NT), np.float32)
    for h in range(8):
        qd = np.exp((j + 1) * lg[h]) * (256.0 ** -0.5)
        kd = np.exp(-(j + 1) * lg[h])
        rope[h, 0] = cos * qd[None, :]
        rope[h, 1] = sin * qd[None, :]
        rope[h, 2] = cos * kd[None, :]
        rope[h, 3] = sin * kd[None, :]
    rc = np.ones((128, 3, 8), np.float32)
    g1024 = np.exp(1024 * lg).astype(np.float32)
    if rank == 2:
        rc[:, 0, :] = g1024[None, :]
    if rank == 3:
        rc[:, 0, :] = g1024[None, :]
        rc[:, 1, :] = g1024[None, :]
    return dict(cst=cst, resetm=resetm, gdec=gdec, rope=rope, rcoef=rc)


def _fm(v):
    return np.ascontiguousarray(v.reshape(-1, 128).T)


def kernel_fused(x, meta_tokens, w_in, ret_gn_g, hg_norm_g, hg_lb_logits, w_ret_out, w_hg_out, w_o,
                 ln1_g, ln1_b, ln2_g, ln2_b, ffn_w_gate, ffn_w_up, ffn_w_down,
                 moe_router, moe_w_gate, moe_w_up, moe_w_down, _cores=(0, 1)):
    f32 = lambda a: np.ascontiguousarray(np.asarray(a, dtype=np.float32))
    x = f32(x)
    NL = _DBG.get("nl", 4)
    consts = [_consts(r) for r in range(4)]
    shared = dict(
        w_in=f32(w_in), lb_logits=np.ascontiguousarray(f32(hg_lb_logits).reshape(4, 16, 128).transpose(2, 0, 1)),
        lbsel=np.ascontiguousarray(np.stack([np.concatenate([np.zeros((128, 1), np.float32),
                                                             np.broadcast_to((np.arange(1, 4) <= l).astype(np.float32)[None, :], (128, 3))], axis=1)
                                             for l in range(4)])),
        rope=np.ascontiguousarray(np.stack([c["rope"] for c in consts])),
        cst=np.ascontiguousarray(np.stack([c["cst"] for c in consts])),
        resetm=consts[0]["resetm"],
        gdec=np.ascontiguousarray(np.stack([c["gdec"] for c in consts])),
        w_ret_out=f32(w_ret_out), w_hg_out=f32(w_hg_out), w_o=f32(w_o),
        gn_g_b=np.ascontiguousarray(np.broadcast_to(f32(ret_gn_g)[:, None, :], (4, 128, 4096))),
        hgn_g=np.ascontiguousarray(np.stack([_fm(f32(hg_norm_g[l])) for l in range(4)])),
        ln=np.ascontiguousarray(np.stack([np.stack([_fm(f32(ln1_g[l])), _fm(f32(ln1_b[l])), _fm(f32(ln2_g[l])), _fm(f32(ln2_b[l]))], axis=1)
                                          for l in range(4)])),
        wr=f32(moe_router), mg=f32(moe_w_gate), mu=f32(moe_w_up), md=f32(moe_w_down),
        wg=f32(ffn_w_gate), wu=f32(ffn_w_up), wd=f32(ffn_w_down))
    in_maps = []
    for ci in _cores:
        b = ci % 2
        hT = np.zeros((4, D, NT), np.float32)
        hT[0, :, 112:128] = f32(meta_tokens).T
        for r in range(4):
            hT[r, :, 128:] = x[b, 1024 * r:1024 * (r + 1)].T
        in_maps.append(dict(shared, hT=hT))
    ncF = _prog("F")
    res = run_bass_kernel_spmd(ncF, in_maps, core_ids=list(range(len(_cores)))).results
    out = np.zeros((2, 4096, D), np.float32)
    for i, ci in enumerate(_cores[:2]):
        b = ci % 2
        ho = np.asarray(res[i]["hT_out"], dtype=np.float32)
        for r in range(4):
            out[b, 1024 * r:1024 * (r + 1)] = ho[r][:, 128:].T
    return out


def kernel_unfused(x, meta_tokens, w_in, ret_gn_g, hg_norm_g, hg_lb_logits, w_ret_out, w_hg_out, w_o,
           ln1_g, ln1_b, ln2_g, ln2_b, ffn_w_gate, ffn_w_up, ffn_w_down,
           moe_router, moe_w_gate, moe_w_up, moe_w_down, _nlayers=4, _debug=None):
    f32 = lambda a: np.ascontiguousarray(np.asarray(a, dtype=np.float32))
    x = f32(x)
    ncore = 8
    cores = [(c // 4, c % 4) for c in range(ncore)]
    consts = [_consts(r) for (_, r) in cores]
    hT = []
    for (b, r) in cores:
        t = np.zeros((D, NT), np.float32)
        if r == 0:
            t[:, 112:128] = f32(meta_tokens).T
        t[:, 128:] = x[b, 1024 * r:1024 * (r + 1)].T
        hT.append(t)
    lbl = np.ascontiguousarray(f32(hg_lb_logits).reshape(4, 16, 128).transpose(2, 0, 1))
    def _lbsel(l):
        t = np.zeros((128, 4), np.float32)
        t[:, 1:l + 1] = 1.0
        return t

    ncA = _prog("A")
    in_maps = []
    for ci in range(ncore):
        cs = consts[ci]
        in_maps.append(dict(w_in=f32(w_in[0]), lb_logits=lbl, lbsel=_lbsel(0), hT=hT[ci], rope=cs["rope"], cst=cs["cst"],
                            resetm=cs["resetm"], gdec=cs["gdec"]))
    resA = run_bass_kernel_spmd(ncA, in_maps, core_ids=list(range(ncore))).results
    for l in range(_nlayers):
        moe = (l % 2 == 1)
        last = (l == _nlayers - 1)
        Bin = dict(w_in=f32(w_in[l]), lb_logits=lbl, lbsel=_lbsel(l),
                   w_ret_out=f32(w_ret_out[l]), w_hg_out=f32(w_hg_out[l]), w_o=f32(w_o[l]),
                   gn_g_b=np.ascontiguousarray(np.broadcast_to(f32(ret_gn_g[l])[None, :], (128, 4096))),
                   hgn_g=_fm(f32(hg_norm_g[l])),
                   ln=np.ascontiguousarray(np.stack([_fm(f32(ln1_g[l])), _fm(f32(ln1_b[l])),
                                                     _fm(f32(ln2_g[l])), _fm(f32(ln2_b[l]))], axis=1)))
        if moe:
            Bin.update(wr=f32(moe_router[l // 2]), mg=f32(moe_w_gate[l // 2]), mu=f32(moe_w_up[l // 2]), md=f32(moe_w_down[l // 2]))
        else:
            Bin.update(wg=f32(ffn_w_gate[l // 2]), wu=f32(ffn_w_up[l // 2]), wd=f32(ffn_w_down[l // 2]))
        if not last:
            Bin.update(w_in_next=f32(w_in[l + 1]), lbsel_next=_lbsel(l + 1))
        in_maps = []
        for ci, (b, r) in enumerate(cores):
            cs = consts[ci]
            Rin = np.zeros((3, 8, 2, 128, 512), np.float32)
            Sin = np.zeros((3, 16, 128, 128), np.float32)
            Lsl = np.zeros((128, 3, 16), np.float32)
            for jr in range(3):
                if jr < r:
                    src = resA[b * 4 + jr]
                    Rin[jr] = src["Rloc"]
                    Sin[jr] = src["Sloc"]
                    if jr >= 1:
                        Lsl[:, jr, :] = src["Lsum"]
            in_maps.append(dict(Bin, hT=hT[ci], rope=cs["rope"], cst=cs["cst"], resetm=cs["resetm"], gdec=cs["gdec"],
                                rcoef=cs["rcoef"], Rin=Rin, Sin=Sin, Lsl=Lsl))
        ncB = _prog("B" if last else "BA", moe)
        resB = run_bass_kernel_spmd(ncB, in_maps, core_ids=list(range(ncore))).results
        hT = [np.asarray(resB[ci]["hT_out"], dtype=np.float32) for ci in range(ncore)]
        resA = resB
        if _debug is not None:
            _debug.append([h.copy() for h in hT])
    out = np.zeros((2, 4096, D), np.float32)
    for ci, (b, r) in enumerate(cores):
        out[b, 1024 * r:1024 * (r + 1)] = hT[ci][:, 128:].T
    return out


FUSED = False


def kernel(**inputs):
    if FUSED:
        return kernel_fused(**inputs)
    return kernel_unfused(**inputs)
```
